# Optimizing a Trainium2 kernel written in Bass

```python
import math
import jax
import jax.numpy as jnp
from jax import lax
import numpy as np

D_MODEL = 1024
BATCH = 8
SEQ = 2048
DEPTH = 2

CHUNK = 64
N_EVEN = (DEPTH + 1) // 2
N_ODD = DEPTH // 2
NORM_EPS = 1e-6

S5_WIDTH = D_MODEL // 2
S5_GROUP = 16
S5_GROUPS = S5_WIDTH // S5_GROUP
S5_STATE = 64

SSD_INNER = D_MODEL
SSD_HEADDIM = 64
SSD_HEADS = SSD_INNER // SSD_HEADDIM
SSD_STATE = 64
SSD_GROUPS = 4
SSD_CONV = 4
SSD_CONV_DIM = SSD_INNER + 2 * SSD_GROUPS * SSD_STATE
EVEN_IN = S5_WIDTH + SSD_INNER + SSD_CONV_DIM + SSD_HEADS
EVEN_MIX = S5_WIDTH + SSD_INNER

RET_HEADS = 4
RET_QK = D_MODEL // 8
RET_V = 2 * RET_QK
RET_KEY_W = RET_HEADS * RET_QK
RET_VAL_W = RET_HEADS * RET_V
ROPE_BASE = 10000.0

GLA_HEADS = 4
GLA_QK = D_MODEL // 8
GLA_V = D_MODEL // 4
GLA_KEY_W = GLA_HEADS * GLA_QK
GLA_VAL_W = GLA_HEADS * GLA_V
GLA_RANK = 16
GLA_TAU = 16.0
ODD_IN = 2 * RET_KEY_W + 2 * RET_VAL_W + 2 * GLA_KEY_W + 2 * GLA_VAL_W + GLA_RANK
ODD_MIX = RET_VAL_W + GLA_VAL_W

FFN_HIDDEN = -(-8 * D_MODEL // (3 * 256)) * 256

kernel_name = "hybrid_s5_ssd_retention_gla_trunk"


def rmsnorm(x, g):
    xf = x.astype(jnp.float32)
    y = xf * lax.rsqrt(jnp.mean(xf * xf, axis=-1, keepdims=True) + NORM_EPS)
    return (y * g.astype(jnp.float32)).astype(x.dtype)


def split_cols(t, sizes):
    parts, start = [], 0
    for s in sizes:
        parts.append(t[..., start:start + s])
        start += s
    return parts


def to_chunks(t):
    bsz, seqlen, nh, d = t.shape
    return t.reshape(bsz, seqlen // CHUNK, CHUNK, nh, d).transpose(1, 0, 3, 2, 4)


def from_chunks(t):
    nc, bsz, nh, c, d = t.shape
    return t.transpose(1, 0, 3, 2, 4).reshape(bsz, nc * c, nh, d)


def causal_depthwise_conv(x, w, b):
    width, ch = w.shape
    y = lax.conv_general_dilated(x, w[:, None, :], window_strides=(1,),
                                 padding=[(width - 1, 0)],
                                 dimension_numbers=('NWC', 'WIO', 'NWC'),
                                 feature_group_count=ch)
    return y + b


def rotary(t):
    seqlen, d = t.shape[1], t.shape[-1]
    half = d // 2
    inv_freq = ROPE_BASE ** (-jnp.arange(half, dtype=jnp.float32) / half)
    ang = jnp.arange(seqlen, dtype=jnp.float32)[:, None] * inv_freq[None, :]
    cos, sin = jnp.cos(ang)[None, :, None, :], jnp.sin(ang)[None, :, None, :]
    t1, t2 = t[..., :half], t[..., half:]
    return jnp.concatenate([t1 * cos - t2 * sin, t1 * sin + t2 * cos], axis=-1)


def s5_combine(left, right):
    al_re, al_im, bl_re, bl_im = left
    ar_re, ar_im, br_re, br_im = right
    return (al_re * ar_re - al_im * ar_im,
            al_re * ar_im + al_im * ar_re,
            ar_re * bl_re - ar_im * bl_im + br_re,
            ar_re * bl_im + ar_im * bl_re + br_im)


def s5_mixer(u, lam_re, lam_im, log_step, b_re, b_im, c_re, c_im, d_skip, glu_w, glu_b):
    f32 = jnp.float32
    bsz, seqlen, _ = u.shape
    ug = u.astype(f32).reshape(bsz, seqlen, S5_GROUPS, S5_GROUP)
    lr, li = lam_re.astype(f32), lam_im.astype(f32)
    step = jnp.exp(log_step.astype(f32))[:, None]
    mag = jnp.exp(lr * step)
    a_re, a_im = mag * jnp.cos(li * step), mag * jnp.sin(li * step)
    den = lr * lr + li * li
    k_re = ((a_re - 1.0) * lr + a_im * li) / den
    k_im = (a_im * lr - (a_re - 1.0) * li) / den
    br, bi = b_re.astype(f32), b_im.astype(f32)
    bb_re = k_re[..., None] * br - k_im[..., None] * bi
    bb_im = k_re[..., None] * bi + k_im[..., None] * br
    bu_re = jnp.einsum('gpn,blgn->lbgp', bb_re, ug)
    bu_im = jnp.einsum('gpn,blgn->lbgp', bb_im, ug)
    shape_a = (seqlen, 1, S5_GROUPS, S5_STATE)
    a_re_t = jnp.broadcast_to(a_re, shape_a)
    a_im_t = jnp.broadcast_to(a_im, shape_a)
    _, _, xs_re, xs_im = lax.associative_scan(s5_combine, (a_re_t, a_im_t, bu_re, bu_im), axis=0)
    y = (jnp.einsum('gnp,lbgp->blgn', c_re.astype(f32), xs_re)
         - jnp.einsum('gnp,lbgp->blgn', c_im.astype(f32), xs_im)
         + d_skip.astype(f32) * ug).reshape(bsz, seqlen, S5_WIDTH)
    z = jax.nn.gelu(y)
    out = z * jax.nn.sigmoid(z @ glu_w.astype(f32) + glu_b.astype(f32))
    return out.astype(u.dtype)


def ssd_mixer(z, xbc, dt_raw, conv_w, conv_b, dt_bias, a_log, d_skip, norm_g):
    f32 = jnp.float32
    bsz, seqlen, _ = z.shape
    nc = seqlen // CHUNK
    xbc = jax.nn.silu(causal_depthwise_conv(xbc.astype(f32), conv_w.astype(f32), conv_b.astype(f32)))
    xs, bm, cm = split_cols(xbc, (SSD_INNER, SSD_GROUPS * SSD_STATE, SSD_GROUPS * SSD_STATE))
    hpg = SSD_HEADS // SSD_GROUPS
    x = xs.reshape(bsz, nc, CHUNK, SSD_HEADS, SSD_HEADDIM)
    bm = jnp.repeat(bm.reshape(bsz, nc, CHUNK, SSD_GROUPS, SSD_STATE), hpg, axis=3)
    cm = jnp.repeat(cm.reshape(bsz, nc, CHUNK, SSD_GROUPS, SSD_STATE), hpg, axis=3)
    dt = jax.nn.softplus(dt_raw.astype(f32) + dt_bias.astype(f32))
    da = (dt * -jnp.exp(a_log.astype(f32))).reshape(bsz, nc, CHUNK, SSD_HEADS).transpose(0, 3, 1, 2)
    xdt = x * dt.reshape(bsz, nc, CHUNK, SSD_HEADS)[..., None]
    a_cum = jnp.cumsum(da, axis=-1)
    causal = jnp.tril(jnp.ones((CHUNK, CHUNK), dtype=bool))
    seg = jnp.exp(jnp.where(causal, a_cum[..., :, None] - a_cum[..., None, :], -jnp.inf))
    scores = jnp.einsum('bclhn,bcshn->bhcls', cm, bm) * seg
    y = jnp.einsum('bhcls,bcshp->bclhp', scores, xdt)
    decay_to_end = jnp.exp(a_cum[..., -1:] - a_cum)
    states = jnp.einsum('bclhn,bhcl,bclhp->cbhpn', bm, decay_to_end, xdt)
    chunk_decay = jnp.exp(a_cum[..., -1]).transpose(2, 0, 1)

    def step(carry, inp):
        st, dec = inp
        return carry * dec[..., None, None] + st, carry

    init = jnp.zeros((bsz, SSD_HEADS, SSD_HEADDIM, SSD_STATE), f32)
    _, prev = lax.scan(step, init, (states, chunk_decay))
    y = y + jnp.einsum('bclhn,cbhpn,bhcl->bclhp', cm, prev, jnp.exp(a_cum))
    y = y + d_skip.astype(f32)[:, None] * x
    y = y.reshape(bsz, seqlen, SSD_INNER) * jax.nn.silu(z.astype(f32))
    return rmsnorm(y, norm_g).astype(z.dtype)


def retention_mixer(q, k, v, g, norm_g):
    f32 = jnp.float32
    bsz, seqlen, _ = q.shape
    qh = rotary(q.astype(f32).reshape(bsz, seqlen, RET_HEADS, RET_QK))
    kh = rotary(k.astype(f32).reshape(bsz, seqlen, RET_HEADS, RET_QK)) * RET_QK ** -0.5
    vh = v.astype(f32).reshape(bsz, seqlen, RET_HEADS, RET_V)
    log_gamma = jnp.log(1.0 - 2.0 ** (-5.0 - jnp.arange(RET_HEADS, dtype=f32)))
    pos = jnp.arange(CHUNK, dtype=f32)
    diff = pos[:, None] - pos[None, :]
    dmat = jnp.where(diff >= 0, jnp.exp(log_gamma[:, None, None] * jnp.maximum(diff, 0.0)), 0.0)
    q_decay = jnp.exp(log_gamma[:, None] * (pos + 1.0))[..., None]
    k_decay = jnp.exp(log_gamma[:, None] * (CHUNK - 1.0 - pos))
    chunk_decay = jnp.exp(log_gamma * CHUNK)[:, None, None]

    def step(state, inp):
        qc, kc, vc = inp
        inner = jnp.einsum('bhid,bhjd->bhij', qc, kc) * dmat
        out = (jnp.einsum('bhij,bhjv->bhiv', inner, vc)
               + jnp.einsum('bhid,bhdv->bhiv', qc, state) * q_decay)
        state = state * chunk_decay + jnp.einsum('bhjd,hj,bhjv->bhdv', kc, k_decay, vc)
        return state, out

    init = jnp.zeros((bsz, RET_HEADS, RET_QK, RET_V), f32)
    _, out = lax.scan(step, init, (to_chunks(qh), to_chunks(kh), to_chunks(vh)))
    o = from_chunks(out)
    mu = jnp.mean(o, axis=-1, keepdims=True)
    var = jnp.mean(jnp.square(o - mu), axis=-1, keepdims=True)
    o = ((o - mu) * lax.rsqrt(var + NORM_EPS)).reshape(bsz, seqlen, RET_VAL_W) * norm_g.astype(f32)
    return (o * jax.nn.silu(g.astype(f32))).astype(q.dtype)


def gla_mixer(q, k, v, a_lr, g, gate_w, gate_b, norm_g):
    f32 = jnp.float32
    bsz, seqlen, _ = q.shape
    log_a = jax.nn.log_sigmoid(a_lr.astype(f32) @ gate_w.astype(f32) + gate_b.astype(f32)) / GLA_TAU
    qh = q.astype(f32).reshape(bsz, seqlen, GLA_HEADS, GLA_QK) * GLA_QK ** -0.5
    kh = k.astype(f32).reshape(bsz, seqlen, GLA_HEADS, GLA_QK)
    vh = v.astype(f32).reshape(bsz, seqlen, GLA_HEADS, GLA_V)
    ah = log_a.reshape(bsz, seqlen, GLA_HEADS, GLA_QK)
    causal = jnp.tril(jnp.ones((CHUNK, CHUNK), dtype=bool))

    def step(state, inp):
        qc, kc, vc, ac = inp
        bcum = jnp.cumsum(ac, axis=2)
        blast = bcum[:, :, -1:, :]
        q_in = qc * jnp.exp(bcum)
        att = jnp.where(causal, jnp.einsum('bhid,bhjd->bhij', q_in, kc * jnp.exp(-bcum)), 0.0)
        out = (jnp.einsum('bhij,bhjv->bhiv', att, vc)
               + jnp.einsum('bhid,bhdv->bhiv', q_in, state))
        state = (state * jnp.exp(blast)[:, :, 0, :, None]
                 + jnp.einsum('bhjd,bhjv->bhdv', kc * jnp.exp(blast - bcum), vc))
        return state, out

    init = jnp.zeros((bsz, GLA_HEADS, GLA_QK, GLA_V), f32)
    _, out = lax.scan(step, init, (to_chunks(qh), to_chunks(kh), to_chunks(vh), to_chunks(ah)))
    o = from_chunks(out)
    o = (o * lax.rsqrt(jnp.mean(o * o, axis=-1, keepdims=True) + NORM_EPS)).reshape(bsz, seqlen, GLA_VAL_W)
    o = o * norm_g.astype(f32)
    return (o * jax.nn.silu(g.astype(f32))).astype(q.dtype)


def swiglu(h, w_gate, w_up, w_down):
    return (jax.nn.silu(h @ w_gate) * (h @ w_up)) @ w_down


def setup_inputs(seed: int = 0) -> dict:
    key = jax.random.key(seed)
    ks = iter(jax.random.split(key, 48))
    f32 = jnp.float32

    def normal(shape, scale):
        return jax.random.normal(next(ks), shape, f32) * scale

    def gain(shape):
        return 1.0 + normal(shape, 0.02)

    def loguniform(shape, lo, hi):
        return jax.random.uniform(next(ks), shape, f32, math.log(lo), math.log(hi))

    x = normal((BATCH, SEQ, D_MODEL), 1.0)
    norm_mix_g = gain((DEPTH, D_MODEL))
    norm_ffn_g = gain((DEPTH, D_MODEL))
    final_norm_g = gain((D_MODEL,))

    ev_in_w = normal((N_EVEN, D_MODEL, EVEN_IN), D_MODEL ** -0.5)
    s5_lam_re = -0.5 + normal((N_EVEN, S5_GROUPS, S5_STATE), 0.01)
    s5_lam_im = (math.pi * jnp.arange(S5_STATE, dtype=f32)) + normal((N_EVEN, S5_GROUPS, S5_STATE), 0.01)
    s5_log_step = loguniform((N_EVEN, S5_GROUPS), 1e-3, 1e-1)
    b_scale = (2.0 * S5_GROUP) ** -0.5
    s5_b_re = normal((N_EVEN, S5_GROUPS, S5_STATE, S5_GROUP), b_scale)
    s5_b_im = normal((N_EVEN, S5_GROUPS, S5_STATE, S5_GROUP), b_scale)
    c_scale = (2.0 * S5_STATE) ** -0.5
    s5_c_re = normal((N_EVEN, S5_GROUPS, S5_GROUP, S5_STATE), c_scale)
    s5_c_im = normal((N_EVEN, S5_GROUPS, S5_GROUP, S5_STATE), c_scale)
    s5_d = normal((N_EVEN, S5_GROUPS, S5_GROUP), 1.0)
    s5_glu_w = normal((N_EVEN, S5_WIDTH, S5_WIDTH), S5_WIDTH ** -0.5)
    s5_glu_b = normal((N_EVEN, S5_WIDTH), 0.02)
    ssd_conv_w = normal((N_EVEN, SSD_CONV, SSD_CONV_DIM), SSD_CONV ** -0.5)
    ssd_conv_b = normal((N_EVEN, SSD_CONV_DIM), 0.02)
    dt0 = jnp.exp(loguniform((N_EVEN, SSD_HEADS), 1e-3, 1e-1))
    ssd_dt_bias = dt0 + jnp.log(-jnp.expm1(-dt0))
    ssd_a_log = jnp.log(jax.random.uniform(next(ks), (N_EVEN, SSD_HEADS), f32, 1.0, 16.0))
    ssd_d = gain((N_EVEN, SSD_HEADS))
    ssd_norm_g = gain((N_EVEN, SSD_INNER))
    ev_out_w = normal((N_EVEN, EVEN_MIX, D_MODEL), EVEN_MIX ** -0.5)

    od_in_w = normal((N_ODD, D_MODEL, ODD_IN), D_MODEL ** -0.5)
    ret_norm_g = gain((N_ODD, RET_VAL_W))
    gla_gate_w = normal((N_ODD, GLA_RANK, GLA_KEY_W), GLA_RANK ** -0.5)
    gla_gate_b = normal((N_ODD, GLA_KEY_W), 0.1)
    gla_norm_g = gain((N_ODD, GLA_VAL_W))
    od_out_w = normal((N_ODD, ODD_MIX, D_MODEL), ODD_MIX ** -0.5)

    ffn_gate_w = normal((DEPTH, D_MODEL, FFN_HIDDEN), D_MODEL ** -0.5)
    ffn_up_w = normal((DEPTH, D_MODEL, FFN_HIDDEN), D_MODEL ** -0.5)
    ffn_down_w = normal((DEPTH, FFN_HIDDEN, D_MODEL), FFN_HIDDEN ** -0.5)

    return {
        "x": x, "norm_mix_g": norm_mix_g, "norm_ffn_g": norm_ffn_g, "final_norm_g": final_norm_g,
        "ev_in_w": ev_in_w, "s5_lam_re": s5_lam_re, "s5_lam_im": s5_lam_im, "s5_log_step": s5_log_step,
        "s5_b_re": s5_b_re, "s5_b_im": s5_b_im, "s5_c_re": s5_c_re, "s5_c_im": s5_c_im, "s5_d": s5_d,
        "s5_glu_w": s5_glu_w, "s5_glu_b": s5_glu_b, "ssd_conv_w": ssd_conv_w, "ssd_conv_b": ssd_conv_b,
        "ssd_dt_bias": ssd_dt_bias, "ssd_a_log": ssd_a_log, "ssd_d": ssd_d, "ssd_norm_g": ssd_norm_g,
        "ev_out_w": ev_out_w, "od_in_w": od_in_w, "ret_norm_g": ret_norm_g, "gla_gate_w": gla_gate_w,
        "gla_gate_b": gla_gate_b, "gla_norm_g": gla_norm_g, "od_out_w": od_out_w,
        "ffn_gate_w": ffn_gate_w, "ffn_up_w": ffn_up_w, "ffn_down_w": ffn_down_w,
    }


def reference(x, norm_mix_g, norm_ffn_g, final_norm_g, ev_in_w, s5_lam_re, s5_lam_im, s5_log_step,
              s5_b_re, s5_b_im, s5_c_re, s5_c_im, s5_d, s5_glu_w, s5_glu_b, ssd_conv_w, ssd_conv_b,
              ssd_dt_bias, ssd_a_log, ssd_d, ssd_norm_g, ev_out_w, od_in_w, ret_norm_g, gla_gate_w,
              gla_gate_b, gla_norm_g, od_out_w, ffn_gate_w, ffn_up_w, ffn_down_w):
    h = x
    for layer in range(DEPTH):
        i = layer // 2
        hn = rmsnorm(h, norm_mix_g[layer])
        if layer % 2 == 0:
            proj = hn @ ev_in_w[i]
            u, z, xbc, dt_raw = split_cols(proj, (S5_WIDTH, SSD_INNER, SSD_CONV_DIM, SSD_HEADS))
            y_a = s5_mixer(u, s5_lam_re[i], s5_lam_im[i], s5_log_step[i], s5_b_re[i], s5_b_im[i],
                           s5_c_re[i], s5_c_im[i], s5_d[i], s5_glu_w[i], s5_glu_b[i])
            y_b = ssd_mixer(z, xbc, dt_raw, ssd_conv_w[i], ssd_conv_b[i], ssd_dt_bias[i],
                            ssd_a_log[i], ssd_d[i], ssd_norm_g[i])
            mix = jnp.concatenate([y_a, y_b], axis=-1) @ ev_out_w[i]
        else:
            proj = hn @ od_in_w[i]
            q_r, k_r, v_r, g_r, q_g, k_g, v_g, a_lr, g_g = split_cols(
                proj, (RET_KEY_W, RET_KEY_W, RET_VAL_W, RET_VAL_W,
                       GLA_KEY_W, GLA_KEY_W, GLA_VAL_W, GLA_RANK, GLA_VAL_W))
            y_c = retention_mixer(q_r, k_r, v_r, g_r, ret_norm_g[i])
            y_d = gla_mixer(q_g, k_g, v_g, a_lr, g_g, gla_gate_w[i], gla_gate_b[i], gla_norm_g[i])
            mix = jnp.concatenate([y_c, y_d], axis=-1) @ od_out_w[i]
        h = h + mix
        hn = rmsnorm(h, norm_ffn_g[layer])
        h = h + swiglu(hn, ffn_gate_w[layer], ffn_up_w[layer], ffn_down_w[layer])
    return rmsnorm(h, final_norm_g)
```

```python
import numpy as np
import ml_dtypes
from concourse.bass_utils import run_bass_kernel_spmd
import numpy as np
import concourse.bass as bass
import concourse.mybir as mybir

F32 = mybir.dt.float32
BF16 = mybir.dt.bfloat16
I32 = mybir.dt.int32
AF = mybir.ActivationFunctionType
ALU = mybir.AluOpType
AX = mybir.AxisListType

COMPUTE = ("pe", "act", "dve", "pool")
QUEUES = ("sp", "act", "pool")
ALLENG = ("pe", "act", "dve", "pool", "sp")


class V:
    __slots__ = ("ap", "keys")

    def __init__(self, ap, keys):
        self.ap = ap
        self.keys = tuple(keys)


class T:
    def __init__(self, name, handle, nsub=None):
        self.name = name
        self.h = handle
        self.nsub = nsub

    def __getitem__(self, idx):
        if self.nsub is None:
            return V(self.h[idx], [(self.name,)])
        return V(self.h[idx], [(self.name, i) for i in range(self.nsub)])

    def s(self, *subs):
        return _Sub(self, subs)


class _Sub:
    def __init__(self, t, subs):
        self.t = t
        self.subs = subs

    def __getitem__(self, idx):
        return V(self.t.h[idx], [(self.t.name, s) for s in self.subs])


def W(v, ap):
    return V(ap, v.keys)


class Op:
    __slots__ = ("eng", "fn", "reads", "writes", "is_dma", "idx", "waits",
                 "signal", "semval", "dsem", "dval", "vc")


class Prog:
    def __init__(self, nc, n_dma_sems=(16, 4, 16)):
        self.nc = nc
        self.ops = []
        self.eng_obj = {"pe": nc.tensor, "act": nc.scalar, "dve": nc.vector,
                        "pool": nc.gpsimd, "sp": nc.sync}
        self.n_dma_sems = dict(zip(QUEUES, n_dma_sems))
        self._stack = []
        self._names = set()
        self._keep = []
        self.cnt = {e: 0 for e in ALLENG}
        self.sc = {e: 0 for e in COMPUTE}
        self.last_writer = {}
        self.readers = {}
        self.dma_rr = {q: 0 for q in QUEUES}
        self.dma_tot = {}
        self.dma_last = {}
        self.evc = {e: {} for e in ALLENG}
        self.sems = {}
        for e in COMPUTE:
            g = nc.semaphore("s_" + e)
            self.sems[e] = g.__enter__()
            self._keep.append(g)
        self.dsems = {}
        for q in QUEUES:
            for s in range(self.n_dma_sems[q]):
                g = nc.semaphore(f"d_{q}_{s}")
                self.dsems[(q, s)] = g.__enter__()
                self._keep.append(g)
        self.n_wait = 0
        self.n_ins = 0

    def _uniq(self, name):
        n = name
        i = 0
        while n in self._names:
            i += 1
            n = f"{name}_{i}"
        self._names.add(n)
        return n

    def sbuf(self, name, shape, dtype, nsub=None):
        name = self._uniq(name)
        g = self.nc.sbuf_tensor(name, list(shape), dtype)
        h = g.__enter__()
        self._stack.append(g)
        return T(name, h, nsub)

    def psum(self, name, shape, dtype, nsub=None):
        name = self._uniq(name)
        g = self.nc.psum_tensor(name, list(shape), dtype)
        h = g.__enter__()
        self._stack.append(g)
        return T(name, h, nsub)

    def dram(self, name, shape, dtype, kind="Internal", nsub=None):
        name = self._uniq(name)
        h = self.nc.dram_tensor(name, list(shape), dtype, kind=kind)
        return T(name, h.ap() if hasattr(h, "ap") else h, nsub)

    def mark(self):
        return len(self._stack)

    def release(self, mark):
        self.flush(barrier=True)
        while len(self._stack) > mark:
            g = self._stack.pop()
            g.__exit__(None, None, None)

    def add(self, eng, fn, reads, writes, is_dma=False):
        op = Op()
        op.eng = eng
        op.fn = fn
        rk = []
        for r in reads:
            if r is None:
                continue
            rk.extend(r.keys)
        wk = []
        for w in writes:
            if w is None:
                continue
            wk.extend(w.keys)
        op.reads = rk
        op.writes = wk
        op.is_dma = is_dma
        self.ops.append(op)
        return op

    def dma(self, out, in_, q="sp", **kw):
        e = self.eng_obj[q]
        return self.add(q, lambda: e.dma_start(out=out.ap, in_=in_.ap, **kw),
                        [in_], [out], is_dma=True)

    def mm(self, out, lhsT, rhs, start=True, stop=True, **kw):
        nc = self.nc
        return self.add("pe", lambda: nc.tensor.matmul(out.ap, lhsT.ap, rhs.ap, start=start, stop=stop, **kw),
                        [lhsT, rhs], [out])

    def transpose(self, out, in_, ident):
        nc = self.nc
        return self.add("pe", lambda: nc.tensor.transpose(out.ap, in_.ap, ident.ap), [in_, ident], [out])

    def act(self, out, in_, func, bias=None, scale=None, accum_out=None):
        nc = self.nc
        kw = {}
        reads = [in_]
        if bias is not None:
            if isinstance(bias, V):
                kw["bias"] = bias.ap
                reads.append(bias)
            else:
                kw["bias"] = bias
        if scale is not None:
            if isinstance(scale, V):
                kw["scale"] = scale.ap
                reads.append(scale)
            else:
                kw["scale"] = scale
        writes = [out]
        if accum_out is not None:
            kw["accum_out"] = accum_out.ap
            writes.append(accum_out)
        return self.add("act", lambda: nc.scalar.activation(out.ap, in_.ap, func, **kw), reads, writes)

    def _veng(self, eng):
        return self.nc.vector if eng == "dve" else self.nc.gpsimd

    def copy(self, out, in_, eng="dve"):
        if eng == "act":
            nc = self.nc
            return self.add("act", lambda: nc.scalar.copy(out.ap, in_.ap), [in_], [out])
        e = self._veng(eng)
        return self.add(eng, lambda: e.tensor_copy(out.ap, in_.ap), [in_], [out])

    def memset(self, out, val, eng="dve"):
        e = self._veng(eng)
        return self.add(eng, lambda: e.memset(out.ap, val), [], [out])

    def tt(self, out, in0, in1, op, eng="dve"):
        e = self._veng(eng)
        return self.add(eng, lambda: e.tensor_tensor(out.ap, in0.ap, in1.ap, op), [in0, in1], [out])

    def ts(self, out, in0, s1, op0, s2=None, op1=None, eng="dve", accum_out=None):
        e = self._veng(eng)
        reads = [in0]
        a1 = s1
        a2 = s2
        if isinstance(s1, V):
            reads.append(s1)
            a1 = s1.ap
        if isinstance(s2, V):
            reads.append(s2)
            a2 = s2.ap
        kw = {}
        writes = [out]
        if op1 is not None:
            kw["op1"] = op1
        if accum_out is not None:
            kw["accum_out"] = accum_out.ap
            writes.append(accum_out)
        return self.add(eng, lambda: e.tensor_scalar(out.ap, in0.ap, a1, a2, op0, **kw), reads, writes)

    def stt(self, out, in0, scalar, in1, op0, op1, eng="dve", accum_out=None):
        e = self._veng(eng)
        reads = [in0, in1]
        a = scalar
        if isinstance(scalar, V):
            reads.append(scalar)
            a = scalar.ap
        kw = {}
        writes = [out]
        if accum_out is not None:
            kw["accum_out"] = accum_out.ap
            writes.append(accum_out)
        return self.add(eng, lambda: e.scalar_tensor_tensor(out.ap, in0.ap, a, in1.ap, op0, op1, **kw), reads, writes)

    def scan(self, out, d0, d1, initial, op0=ALU.mult, op1=ALU.add):
        nc = self.nc
        reads = [d0, d1]
        a = initial
        if isinstance(initial, V):
            reads.append(initial)
            a = initial.ap
        return self.add("dve", lambda: nc.vector.tensor_tensor_scan(out.ap, d0.ap, d1.ap, a, op0, op1), reads, [out])

    def recip(self, out, in_):
        nc = self.nc
        return self.add("dve", lambda: nc.vector.reciprocal(out.ap, in_.ap), [in_], [out])

    def _done_clock(self, op):
        if op.is_dma:
            return (("d",) + op.dsem, op.dval)
        return (op.eng, op.idx)

    def flush(self, barrier=False, final_wait_ops=()):
        nc = self.nc
        ops = self.ops
        self.ops = []
        last_writer = self.last_writer
        readers = self.readers
        for op in ops:
            self.cnt[op.eng] += 1
            op.idx = self.cnt[op.eng]
            op.waits = []
            op.signal = False
            vc = self.evc[op.eng]
            deps = []
            for k in op.reads:
                w = last_writer.get(k)
                if w is not None:
                    deps.append((w, "raw"))
            for k in op.writes:
                w = last_writer.get(k)
                if w is not None:
                    deps.append((w, "waw"))
                for r in readers.get(k, ()):
                    deps.append((r, "war"))
            if op.is_dma:
                q = op.eng
                slot = self.dma_rr[q] % self.n_dma_sems[q]
                self.dma_rr[q] += 1
                op.dsem = (q, slot)
                prev = self.dma_last.get((q, slot))
                if prev is not None:
                    deps.append((prev, "sem"))
                self.dma_tot[(q, slot)] = self.dma_tot.get((q, slot), 0) + 1
                op.dval = self.dma_tot[(q, slot)]
                self.dma_last[(q, slot)] = op
            seen = set()
            for d, kind in deps:
                if d is op or id(d) in seen:
                    continue
                seen.add(id(d))
                if (not d.is_dma) and (not op.is_dma) and d.eng == op.eng:
                    if kind == "waw" and d.eng == "pe":
                        continue
                cn, cv = self._done_clock(d)
                if vc.get(cn, 0) >= cv:
                    continue
                op.waits.append(d)
                vc[cn] = cv
                for n2, v2 in d.vc.items():
                    if vc.get(n2, 0) < v2:
                        vc[n2] = v2
                if not d.is_dma:
                    d.signal = True
            op.vc = dict(vc)
            for k in op.reads:
                readers.setdefault(k, []).append(op)
            for k in op.writes:
                last_writer[k] = op
                readers[k] = []
        fin = list(final_wait_ops)
        for d in fin:
            if not d.is_dma:
                d.signal = True
        for op in ops:
            if op.is_dma:
                continue
            if op.signal:
                self.sc[op.eng] += 1
            op.semval = self.sc[op.eng]
        for op in ops:
            e = self.eng_obj[op.eng]
            for d in op.waits:
                if d.is_dma:
                    e.wait_ge(self.dsems[d.dsem], 16 * d.dval)
                else:
                    e.wait_ge(self.sems[d.eng], d.semval)
                self.n_wait += 1
            ins = op.fn()
            self.n_ins += 1
            if op.is_dma:
                ins.then_inc(self.dsems[op.dsem], 16)
            elif op.signal:
                ins.then_inc(self.sems[op.eng], 1)
        for d in fin:
            if d.is_dma:
                nc.sync.wait_ge(self.dsems[d.dsem], 16 * d.dval)
            else:
                nc.sync.wait_ge(self.sems[d.eng], d.semval)
        if barrier:
            for e in COMPUTE:
                self.eng_obj[e].drain().then_inc(self.sems[e], 1)
                self.sc[e] += 1
            for e in ALLENG:
                eo = self.eng_obj[e]
                for e2 in COMPUTE:
                    if e2 != e:
                        eo.wait_ge(self.sems[e2], self.sc[e2])
                for (q, s), tot in self.dma_tot.items():
                    eo.wait_ge(self.dsems[(q, s)], 16 * tot)
            for e in ALLENG:
                vc = self.evc[e]
                for e2 in COMPUTE:
                    vc[e2] = self.cnt[e2]
                for (q, s), tot in self.dma_tot.items():
                    vc[("d", q, s)] = tot
            self.last_writer = {}
            self.readers = {}

    def close(self):
        while self._stack:
            g = self._stack.pop()
            g.__exit__(None, None, None)
        while self._keep:
            g = self._keep.pop()
            g.__exit__(None, None, None)


L = 2048
D = 1024
NT = L // 128
KT = D // 128
FF = 2816
FT = FF // 128
EPS = 1e-6
EVEN_IN = 3088
ODD_IN = 6160

WEIGHT_NAMES = ["norm_mix_g", "norm_ffn_g", "final_norm_g", "ev_in_w", "s5_lam_re", "s5_lam_im", "s5_log_step",
                "s5_b_re", "s5_b_im", "s5_c_re", "s5_c_im", "s5_d", "s5_glu_w", "s5_glu_b", "ssd_conv_w",
                "ssd_conv_b", "ssd_dt_bias", "ssd_a_log", "ssd_d", "ssd_norm_g", "ev_out_w", "od_in_w",
                "ret_norm_g", "gla_gate_w", "gla_gate_b", "gla_norm_g", "od_out_w", "ffn_gate_w", "ffn_up_w",
                "ffn_down_w"]


def host_consts():
    c = {}
    c["c_ident"] = np.eye(128, dtype=np.float32)
    k = np.arange(128)
    c["c_tri"] = (k[:, None] <= k[None, :]).astype(np.float32)
    c["c_up"] = (k[:, None] > k[None, :]).astype(np.float32)
    c["c_pos"] = np.broadcast_to(np.arange(L, dtype=np.float32)[None, :], (128, L)).copy()
    c["c_pidx"] = np.arange(128, dtype=np.float32)[:, None].copy()
    c["c_dmod"] = (np.arange(128) % 64).astype(np.float32)[:, None].copy()
    c["c_sign"] = np.where(np.arange(128) < 64, -1.0, 1.0).astype(np.float32)[:, None].copy()
    c["c_gmask"] = (np.arange(128)[:, None] // 16 == np.arange(8)[None, :]).astype(np.float32)
    return c


class Ctx:
    pass


def rearr(t, pattern, **kw):
    return V(t.h.rearrange(pattern, **kw), [(t.name,)])


def build(taps=(), stop_after=None, skip_mix=False, dbg=0, skip_l0=False):
    nc = bass.Bass("TRN2", target_bir_lowering=False)
    P = Prog(nc)
    C = Ctx()
    C.P = P
    C.nc = nc
    spec = {"x": [L, D], "norm_mix_g": [2, D], "norm_ffn_g": [2, D], "final_norm_g": [D],
            "ev_in_w": [1, D, EVEN_IN], "s5_lam_re": [1, 32, 64], "s5_lam_im": [1, 32, 64], "s5_log_step": [1, 32],
            "s5_b_re": [1, 32, 64, 16], "s5_b_im": [1, 32, 64, 16], "s5_c_re": [1, 32, 16, 64],
            "s5_c_im": [1, 32, 16, 64], "s5_d": [1, 32, 16], "s5_glu_w": [1, 512, 512], "s5_glu_b": [1, 512],
            "ssd_conv_w": [1, 4, 1536], "ssd_conv_b": [1, 1536], "ssd_dt_bias": [1, 16], "ssd_a_log": [1, 16],
            "ssd_d": [1, 16], "ssd_norm_g": [1, 1024], "ev_out_w": [1, 1536, D], "od_in_w": [1, D, ODD_IN],
            "ret_norm_g": [1, 1024], "gla_gate_w": [1, 16, 512], "gla_gate_b": [1, 512], "gla_norm_g": [1, 1024],
            "od_out_w": [1, 2048, D], "ffn_gate_w": [2, D, FF], "ffn_up_w": [2, D, FF], "ffn_down_w": [2, FF, D]}
    I = {}
    for n, s in spec.items():
        I[n] = P.dram(n, s, F32, kind="ExternalInput")
    for n, a in host_consts().items():
        I[n] = P.dram(n, list(a.shape), F32, kind="ExternalInput")
    C.I = I
    out = P.dram("out", [L, D], F32, kind="ExternalOutput")
    C.taps = {}
    C.tap_req = taps
    C.stop_after = stop_after
    C.dbg = dbg

    C.hT = P.sbuf("hT", [128, KT, L], F32, nsub=NT)
    C.identf = P.sbuf("identf", [128, 128], F32)
    C.identb = P.sbuf("identb", [128, 128], BF16)
    C.onesb = P.sbuf("onesb", [128, 128], BF16)
    C.gcols = P.sbuf("gcols", [128, 5, KT], F32)
    P.dma(C.identf[:], I["c_ident"][:])
    P.copy(C.identb[:], C.identf[:])
    P.memset(C.onesb[:], 1.0)
    for i in range(2):
        P.dma(C.gcols[:, i, :], V(I["norm_mix_g"].h.rearrange("l (k p) -> l p k", p=128)[i], [("norm_mix_g",)]), allow_slow_non_contiguous=True)
        P.dma(C.gcols[:, 2 + i, :], V(I["norm_ffn_g"].h.rearrange("l (k p) -> l p k", p=128)[i], [("norm_ffn_g",)]), allow_slow_non_contiguous=True)
    P.dma(C.gcols[:, 4, :], rearr(I["final_norm_g"], "(k p) -> p k", p=128), allow_slow_non_contiguous=True)

    load_x(C)
    if stop_after == "load":
        return finish(C, out)
    for layer in range(2):
        if not skip_mix:
            if layer == 0:
                if skip_l0:
                    continue
                layer0_mixer(C)
                if "h0" in taps:
                    t_ = P.dram("tap_h0", [128, KT, L], F32, kind="ExternalOutput")
                    P.dma(t_[:], C.hT[:])
                    P.flush(barrier=True)
                if stop_after in ("A", "s5", "s5setup", "ssd", "mix0"):
                    return finish(C, out)
            else:
                layer1_mixer(C)
                if "h1" in taps:
                    t_ = P.dram("tap_h1", [128, KT, L], F32, kind="ExternalOutput")
                    P.dma(t_[:], C.hT[:])
                    P.flush(barrier=True)
                if stop_after in ("mix1", "l1mix"):
                    return finish(C, out)
        ffn(C, layer)
    final(C, out)
    return finish(C, out, done=True)


def finish(C, out, done=False):
    P = C.P
    P.flush(barrier=True)
    return C


def hT_v(C, k, t0, n):
    subs = list(range(t0 // 128, (t0 + n + 127) // 128))
    if k is None:
        return C.hT.s(*subs)[:, :, t0:t0 + n]
    return C.hT.s(*subs)[:, k, t0:t0 + n]


def load_x(C):
    P, I = C.P, C.I
    m = P.mark()
    xt = [P.sbuf("xt", [128, D], F32) for _ in range(3)]
    ps = [P.psum("pst", [128, 4, 128], F32) for _ in range(4)]
    n = 0
    for t in range(NT):
        xb = xt[t % 3]
        P.dma(xb[:], I["x"][t * 128:(t + 1) * 128, :])
        for half in range(2):
            pp = ps[n % 4]
            n += 1
            for j in range(4):
                k = half * 4 + j
                P.transpose(pp[:, j, :], xb[:, k * 128:(k + 1) * 128], C.identf[:])
            dst = C.hT.s(t)[:, half * 4:(half + 1) * 4, t * 128:(t + 1) * 128]
            if half == 0:
                P.copy(dst, pp[:], eng="dve")
            else:
                P.copy(dst, pp[:], eng="act")
    P.release(m)


def norm_T(C, gi, hnT):
    P = C.P
    sq = [P.sbuf("sq", [128, KT, 512], BF16) for _ in range(2)]
    ssp = [P.psum("ssp", [128, 512], F32) for _ in range(2)]
    rs = [P.sbuf("rs", [128, 512], F32) for _ in range(2)]
    for b in range(L // 512):
        t0 = b * 512
        s = sq[b % 2]
        pp = ssp[b % 2]
        r = rs[b % 2]
        P.act(s[:], hT_v(C, None, t0, 512), AF.Square)
        for k in range(KT):
            P.mm(pp[:], C.onesb[:], s[:, k, :], start=(k == 0), stop=(k == KT - 1))
        P.act(r[:], pp[:], AF.Sqrt, bias=EPS, scale=1.0 / D)
        P.recip(r[:], r[:])
        for k in range(KT):
            P.stt(hnT.s(b)[:, k, t0:t0 + 512], hT_v(C, k, t0, 512), C.gcols[:, gi, k:k + 1], r[:],
                  ALU.mult, ALU.mult)


def load_w_tile(C, buf, wT, k_tiles, c0, ncols, q="pool"):
    src = V(wT.h.rearrange("(k p) f -> p k f", p=128)[:, :, c0:c0 + ncols], [(wT.name,)])
    C.P.dma(buf[:, 0:k_tiles, 0:ncols], src, q=q)


def dram2d(t, idx):
    return T(t.name, t.h[idx])


def ffn(C, layer):
    P, I = C.P, C.I
    m = P.mark()
    hnT = P.sbuf("hnT", [128, KT, L], BF16, nsub=L // 512)
    m2 = P.mark()
    norm_T(C, 2 + layer, hnT)
    P.release(m2)
    Wg = dram2d(I["ffn_gate_w"], layer)
    Wu = dram2d(I["ffn_up_w"], layer)
    Wd = dram2d(I["ffn_down_w"], layer)
    HALF = 1024
    aT = P.sbuf("aT", [128, FT, HALF], BF16, nsub=FT)
    wg = [P.sbuf("wg", [128, KT, 128], BF16) for _ in range(3)]
    wu = [P.sbuf("wu", [128, KT, 128], BF16) for _ in range(3)]
    wd = [P.sbuf("wd", [128, FT, 128], BF16) for _ in range(2)]
    gps = [P.psum("gps", [128, 512], F32) for _ in range(2)]
    ups = [P.psum("ups", [128, 512], F32) for _ in range(2)]
    dps = [P.psum("dps", [128, 512], F32) for _ in range(2)]
    sg = [P.sbuf("sg", [128, 512], F32) for _ in range(2)]
    n = 0
    nd = 0
    for half in range(L // HALF):
        for ft in range(FT):
            g = wg[ft % 3]
            u = wu[ft % 3]
            load_w_tile(C, g, Wg, KT, ft * 128, 128)
            load_w_tile(C, u, Wu, KT, ft * 128, 128)
            for blk in range(HALF // 512):
                t0 = half * HALF + blk * 512
                b = t0 // 512
                gp = gps[n % 2]
                up = ups[n % 2]
                s = sg[n % 2]
                n += 1
                for k in range(KT):
                    P.mm(gp[:], g[:, k, :], hnT.s(b)[:, k, t0:t0 + 512], start=(k == 0), stop=(k == KT - 1))
                for k in range(KT):
                    P.mm(up[:], u[:, k, :], hnT.s(b)[:, k, t0:t0 + 512], start=(k == 0), stop=(k == KT - 1))
                P.act(s[:], gp[:], AF.Silu)
                P.tt(aT.s(ft)[:, ft, blk * 512:(blk + 1) * 512], up[:], s[:], ALU.mult)
        for dt_ in range(KT):
            w = wd[dt_ % 2]
            load_w_tile(C, w, Wd, FT, dt_ * 128, 128)
            for blk in range(HALF // 512):
                t0 = half * HALF + blk * 512
                dp = dps[nd % 2]
                nd += 1
                for f in range(FT):
                    P.mm(dp[:], w[:, f, :], aT.s(f)[:, f, blk * 512:(blk + 1) * 512], start=(f == 0), stop=(f == FT - 1))
                hv = hT_v(C, dt_, t0, 512)
                P.tt(hv, dp[:], hv, ALU.add)
    P.release(m)


def final(C, out):
    P = C.P
    m = P.mark()
    sq = [P.sbuf("fsq", [128, KT, 512], BF16) for _ in range(2)]
    ssp = [P.psum("fssp", [128, 512], F32) for _ in range(2)]
    rs = [P.sbuf("frs", [128, 512], F32) for _ in range(2)]
    yn = [P.sbuf("fyn", [128, KT, 512], F32) for _ in range(2)]
    tp = [P.psum("ftp", [128, 4, 128], F32) for _ in range(4)]
    ot = [P.sbuf("fot", [128, D], F32) for _ in range(3)]
    n = 0
    outs = []
    for b in range(L // 512):
        t0 = b * 512
        s = sq[b % 2]
        pp = ssp[b % 2]
        r = rs[b % 2]
        y = yn[b % 2]
        P.act(s[:], hT_v(C, None, t0, 512), AF.Square)
        for k in range(KT):
            P.mm(pp[:], C.onesb[:], s[:, k, :], start=(k == 0), stop=(k == KT - 1))
        P.act(r[:], pp[:], AF.Sqrt, bias=EPS, scale=1.0 / D)
        P.recip(r[:], r[:])
        for k in range(KT):
            P.stt(y[:, k, :], hT_v(C, k, t0, 512), C.gcols[:, 4, k:k + 1], r[:], ALU.mult, ALU.mult)
        for tt_ in range(4):
            o = ot[(b * 4 + tt_) % 3]
            for half in range(2):
                pq = tp[n % 4]
                n += 1
                for j in range(4):
                    k = half * 4 + j
                    P.transpose(pq[:, j, :], y[:, k, tt_ * 128:(tt_ + 1) * 128], C.identf[:])
                dst = W(o[:], o.h[:, half * 512:(half + 1) * 512].rearrange("p (j c) -> p j c", j=4))
                if half == 0:
                    P.copy(dst, pq[:], eng="dve")
                else:
                    P.copy(dst, pq[:], eng="act")
            tok = t0 + tt_ * 128
            outs.append(P.dma(out[tok:tok + 128, :], o[:]))
    C.out_ops = outs
    P.release(m)


def make_in_maps(inputs, n_cores=8):
    consts = host_consts()
    maps = []
    for b in range(n_cores):
        mp = {"x": np.ascontiguousarray(inputs["x"][b])}
        for n in WEIGHT_NAMES:
            mp[n] = np.ascontiguousarray(inputs[n])
        mp.update(consts)
        maps.append(mp)
    return maps


_CACHE = {}


def kernel(**inputs):
    inputs = {k: np.asarray(v) for k, v in inputs.items()}
    if "nc" not in _CACHE:
        C = build()
        _CACHE["nc"] = C.nc
    nc = _CACHE["nc"]
    maps = make_in_maps(inputs, 8)
    res = run_bass_kernel_spmd(nc, maps, core_ids=list(range(8)))
    return np.stack([res.results[b]["out"] for b in range(8)], axis=0).astype(np.float32)

import math
TWO_PI = 2.0 * math.pi


def pipeline(n, phases):
    K = len(phases)
    for step in range(n + K - 1):
        for k in range(K - 1, -1, -1):
            i = step - k
            if 0 <= i < n:
                phases[k](i)


def load_w(C, dstV, wT, c0, ncols, q="pool"):
    src = V(wT.h.rearrange("(k p) f -> p k f", p=128)[:, :, c0:c0 + ncols], [(wT.name,)])
    C.P.dma(dstV, src, q=q)


def tap(C, name, srcV, shape, dtype):
    if name not in C.tap_req:
        return
    t = C.P.dram("tap_" + name, shape, dtype, kind="ExternalOutput")
    C.P.dma(t[:], srcV)


def range_reduce(C, x, tmp_f, tmp_i, out):
    P = C.P
    P.ts(tmp_f, x, 1.0 / TWO_PI, ALU.mult, None)
    P.copy(tmp_i, tmp_f)
    P.copy(tmp_f, tmp_i)
    P.stt(out, tmp_f, -TWO_PI, x, ALU.mult, ALU.add)
    P.ts(tmp_f, out, math.pi, ALU.is_gt, None)
    P.stt(out, tmp_f, -TWO_PI, out, ALU.mult, ALU.add)
    P.ts(tmp_f, out, -math.pi, ALU.is_lt, None)
    P.stt(out, tmp_f, TWO_PI, out, ALU.mult, ALU.add)


def layer0_mixer(C):
    P, I = C.P, C.I
    m0 = P.mark()
    uT = P.sbuf("uT", [128, 4, L], BF16, nsub=4)
    mixT = P.dram("d_mixT", [1536, L], BF16, nsub=4)
    dtda = P.sbuf("dtda", [128, NT, 32], F32, nsub=NT)
    d_zs = P.dram("d_zs", [L, 1024], BF16, nsub=NT)
    d_x = P.dram("d_x", [L, 1024], BF16, nsub=4)
    d_B = P.dram("d_B", [L, 256], BF16, nsub=4)
    d_BT = P.dram("d_BT", [256, L], BF16, nsub=4)
    d_CT = P.dram("d_CT", [256, L], BF16, nsub=4)
    C.uT, C.mixT, C.dtda = uT, mixT, dtda
    C.d_zs, C.d_x, C.d_B, C.d_BT, C.d_CT = d_zs, d_x, d_B, d_BT, d_CT
    Win = dram2d(I["ev_in_w"], 0)

    mA = P.mark()
    hnT = P.sbuf("hnT0", [128, KT, L], BF16, nsub=4)
    m2 = P.mark()
    norm_T(C, 0, hnT)
    P.release(m2)

    m2 = P.mark()
    wbuf = [P.sbuf("wA", [128, KT, 128], BF16) for _ in range(3)]
    pA = [P.psum("pA", [128, 512], F32) for _ in range(2)]
    n = 0
    wnat = [P.sbuf("wnat", [128, KT, 128], BF16) for _ in range(2)]
    for ft in range(4):
        w = wbuf[ft % 3]
        wn = wnat[ft % 2]
        load_w(C, wn[:], Win, ft * 128, 128)
        for h in range(2):
            dst = W(w[:], w.h[:, :, h * 64:(h + 1) * 64].rearrange("p k (gl m) -> p k gl m", gl=4, m=16))
            srcv = W(wn[:], wn.h[:, :, :].rearrange("p k (gl h m) -> p k gl h m", gl=4, h=2, m=16)[:, :, :, h, :])
            P.copy(dst, srcv, eng="pool")
        for b in range(4):
            pp = pA[n % 2]
            n += 1
            for k in range(KT):
                P.mm(pp[:], w[:, k, :], hnT.s(b)[:, k, b * 512:(b + 1) * 512], start=(k == 0), stop=(k == KT - 1))
            P.copy(uT.s(b)[:, ft, b * 512:(b + 1) * 512], pp[:], eng="act")
    P.release(m2)

    m2 = P.mark()
    wz = [P.sbuf("wz", [128, KT, 512], BF16) for _ in range(2)]
    load_w(C, wz[0][:], Win, 512, 512)
    load_w(C, wz[1][:], Win, 1024, 512)
    zst = [P.sbuf("zst", [128, 1024], BF16) for _ in range(2)]
    pz = [P.psum("pz", [128, 512], F32) for _ in range(4)]
    n = 0
    for t in range(NT):
        st = zst[t % 2]
        for c in range(2):
            pp = pz[n % 4]
            n += 1
            for k in range(KT):
                P.mm(pp[:], hnT.s(t // 4)[:, k, t * 128:(t + 1) * 128], wz[c][:, k, :], start=(k == 0), stop=(k == KT - 1))
            P.act(st[:, c * 512:(c + 1) * 512], pp[:], AF.Silu)
        P.dma(d_zs.s(t)[t * 128:(t + 1) * 128, :], st[:])
    P.release(m2)

    m2 = P.mark()
    wdt = P.sbuf("wdt", [128, KT, 16], BF16)
    load_w(C, wdt[:], Win, 3072, 16)
    dtb = P.sbuf("dtb", [128, 16], F32)
    negA = P.sbuf("negA", [128, 16], F32)
    P.dma(dtb[:], V(I["ssd_dt_bias"].h[0].partition_broadcast(128), [("ssd_dt_bias",)]))
    P.dma(negA[:], V(I["ssd_a_log"].h[0].partition_broadcast(128), [("ssd_a_log",)]))
    P.act(negA[:], negA[:], AF.Exp)
    P.ts(negA[:], negA[:], -1.0, ALU.mult, None)
    pdt = [P.psum("pdt", [128, 16], F32) for _ in range(2)]
    dtt = [P.sbuf("dtt", [128, 16], F32) for _ in range(2)]
    for t in range(NT):
        pp = pdt[t % 2]
        tmp = dtt[t % 2]
        for k in range(KT):
            P.mm(pp[:], hnT.s(t // 4)[:, k, t * 128:(t + 1) * 128], wdt[:, k, :], start=(k == 0), stop=(k == KT - 1))
        P.tt(tmp[:], pp[:], dtb[:], ALU.add)
        P.act(tmp[:], tmp[:], AF.Exp)
        P.act(dtda.s(t)[:, t, 0:16], tmp[:], AF.Ln, bias=1.0)
        P.tt(dtda.s(t)[:, t, 16:32], dtda.s(t)[:, t, 0:16], negA[:], ALU.mult)
    P.release(m2)

    m2 = P.mark()
    wx = P.sbuf("wx", [128, KT, 1536], BF16)
    for j in range(3):
        load_w(C, wx[:, :, j * 512:(j + 1) * 512], Win, 1536 + j * 512, 512)
    cw = P.sbuf("cw", [128, 12, 4], F32)
    cb = P.sbuf("cb", [128, 12], F32)
    for k in range(4):
        P.dma(cw[:, :, k], V(I["ssd_conv_w"].h[0, k].rearrange("(t p) -> p t", p=128), [("ssd_conv_w",)]),
              allow_slow_non_contiguous=True)
    P.dma(cb[:], V(I["ssd_conv_b"].h[0].rearrange("(t p) -> p t", p=128), [("ssd_conv_b",)]),
          allow_slow_non_contiguous=True)
    xr = P.sbuf("xr", [128, 12, 515], F32, nsub=12)
    P.memset(xr[:], 0.0)
    acc = [P.sbuf("acc", [128, 512], F32) for _ in range(2)]
    xc = [P.sbuf("xc", [128, 512], BF16) for _ in range(3)]
    stg = [P.sbuf("stg", [128, 4, 1280], BF16) for _ in range(2)]
    ptx = [P.psum("ptx", [128, 4, 128], BF16) for _ in range(2)]
    px = [P.psum("px", [128, 512], F32) for _ in range(2)]
    def q0(it):
        b, ft = it // 12, it % 12
        pp = px[it % 2]
        for k in range(KT):
            P.mm(pp[:], wx[:, k, ft * 128:(ft + 1) * 128], hnT.s(b)[:, k, b * 512:(b + 1) * 512],
                 start=(k == 0), stop=(k == KT - 1))

    def q1(it):
        b, ft = it // 12, it % 12
        pp = px[it % 2]
        a = acc[it % 2]
        if b > 0:
            P.copy(xr.s(ft)[:, ft, 0:3], xr.s(ft)[:, ft, 512:515])
        P.copy(xr.s(ft)[:, ft, 3:515], pp[:], eng="act")
        P.act(a[:], xr.s(ft)[:, ft, 0:512], AF.Identity, bias=cb[:, ft:ft + 1], scale=cw[:, ft, 0:1])

    def q2(it):
        b, ft = it // 12, it % 12
        a = acc[it % 2]
        x_ = xc[it % 3]
        for k in range(1, 4):
            P.stt(a[:], xr.s(ft)[:, ft, k:k + 512], cw[:, ft, k:k + 1], a[:], ALU.mult, ALU.add)
        P.act(x_[:], a[:], AF.Silu)

    def q3(it):
        b, ft = it // 12, it % 12
        x_ = xc[it % 3]
        pt = ptx[it % 2]
        sg_ = stg[b % 2]
        if ft < 10:
            for j in range(4):
                P.transpose(pt[:, j, :], x_[:, j * 128:(j + 1) * 128], C.identb[:])
            P.copy(sg_[:, :, ft * 128:(ft + 1) * 128], pt[:])
        if ft >= 8:
            dst = d_BT if ft < 10 else d_CT
            r0 = (ft % 2) * 128
            P.dma(dst.s(b)[r0:r0 + 128, b * 512:(b + 1) * 512], x_[:])
        if ft == 11:
            P.dma(V(d_x.h[b * 512:(b + 1) * 512, :].rearrange("(j p) c -> p j c", p=128), [(d_x.name, b)]),
                  sg_[:, :, 0:1024])
            P.dma(V(d_B.h[b * 512:(b + 1) * 512, :].rearrange("(j p) c -> p j c", p=128), [(d_B.name, b)]),
                  sg_[:, :, 1024:1280])

    pipeline(48, [q0, q1, q2, q3])
    P.release(m2)
    P.release(mA)
    if "A" in C.tap_req:
        tap(C, "A", uT[:], [128, 4, L], BF16)
        tap(C, "A", dtda[:], [128, NT, 32], F32) if False else None
        t2 = P.dram("tap_dtda", [128, NT, 32], F32, kind="ExternalOutput")
        P.dma(t2[:], dtda[:])
        for nm, src, shp in (("zs", d_zs, [L, 1024]), ("x", d_x, [L, 1024]), ("B", d_B, [L, 256]), ("BT", d_BT, [256, L]), ("CT", d_CT, [256, L])):
            t3 = P.dram("tap_" + nm, shp, BF16, kind="ExternalOutput")
            P.dma(t3[:], src[:])
        P.flush(barrier=True)
    if C.stop_after == "A":
        P.release(m0)
        return

    if C.dbg != 99:
        s5_stage(C)
    P.flush(barrier=True)
    if "ya" in C.tap_req:
        tap(C, "ya", mixT[0:512, :], [512, L], BF16)
        P.flush(barrier=True)
    if C.stop_after in ("s5", "s5setup"):
        P.release(m0)
        return
    wo0 = out_proj_load(C, dram2d(I["ev_out_w"], 0), 12, s5_perm=True)
    ssd_stage(C)
    P.flush(barrier=True)
    if "yb" in C.tap_req:
        tap(C, "yb", mixT[512:1536, :], [1024, L], BF16)
        P.flush(barrier=True)
    if C.stop_after == "ssd":
        P.release(m0)
        return
    out_proj_run(C, wo0, 12)
    P.release(m0)


def out_proj_load(C, Wout, nk, s5_perm=False):
    P = C.P
    wo = P.sbuf("wo", [128, nk, D], BF16)
    src = Wout.h.rearrange("(k p) f -> p k f", p=128)
    if s5_perm:
        for k in range(4, nk):
            P.dma(wo[:, k, :], V(src[:, k, :], [(Wout.name,)]), q="pool")
        for k in range(4):
            for h in range(2):
                for gl in range(4):
                    r0 = k * 128 + gl * 32 + h * 16
                    P.dma(wo[h * 64 + gl * 16:h * 64 + gl * 16 + 16, k, :],
                          V(Wout.h[r0:r0 + 16, :], [(Wout.name,)]), q="pool")
    else:
        for k in range(nk):
            P.dma(wo[:, k, :], V(src[:, k, :], [(Wout.name,)]), q="pool")
    return wo


def out_proj_run(C, wo, nk):
    P = C.P
    m = P.mark()
    mixT = C.mixT
    mb = [P.sbuf("mb", [128, nk, 512], BF16) for _ in range(2)]
    po = [P.psum("po", [128, 512], F32) for _ in range(2)]
    n = 0
    for b in range(4):
        mb_ = mb[b % 2]
        P.dma(mb_[:], V(mixT.h.rearrange("(k p) t -> p k t", p=128)[:, :, b * 512:(b + 1) * 512], [(mixT.name, b)]))
        for dt_ in range(KT):
            pp = po[n % 2]
            n += 1
            for k in range(nk):
                P.mm(pp[:], wo[:, k, dt_ * 128:(dt_ + 1) * 128], mb_[:, k, :], start=(k == 0), stop=(k == nk - 1))
            hv = hT_v(C, dt_, b * 512, 512)
            P.tt(hv, pp[:], hv, ALU.add)
    P.release(m)


def s5_stage(C):
    P, I = C.P, C.I
    uT, mixT = C.uT, C.mixT
    m = P.mark()
    def pt(name, n=16):
        return P.sbuf(name, [128, n], F32)
    lr, li, stp = pt("lr"), pt("li"), pt("stp")
    for h in range(2):
        P.dma(lr[h * 64:(h + 1) * 64, :], V(I["s5_lam_re"].h[0].rearrange("(G h) p -> h p G", h=2)[h], [("s5_lam_re",)]),
              allow_slow_non_contiguous=True)
        P.dma(li[h * 64:(h + 1) * 64, :], V(I["s5_lam_im"].h[0].rearrange("(G h) p -> h p G", h=2)[h], [("s5_lam_im",)]),
              allow_slow_non_contiguous=True)
        P.dma(stp[h * 64:(h + 1) * 64, :],
              V(I["s5_log_step"].h[0].rearrange("(G h) -> h G", h=2)[h].partition_broadcast(64), [("s5_log_step",)]),
              allow_slow_non_contiguous=True)
    P.act(stp[:], stp[:], AF.Exp)
    lrs, th, mag = pt("lrs"), pt("th"), pt("mag")
    P.tt(lrs[:], lr[:], stp[:], ALU.mult)
    P.tt(th[:], li[:], stp[:], ALU.mult)
    P.act(mag[:], lrs[:], AF.Exp)
    tf, rr_, thc = pt("tf"), pt("rr"), pt("thc")
    ti = P.sbuf("ti", [128, 16], I32)
    cs, sn = pt("cs"), pt("sn")
    range_reduce(C, th[:], tf[:], ti[:], rr_[:])
    P.act(sn[:], rr_[:], AF.Sin)
    P.ts(thc[:], th[:], math.pi / 2, ALU.add, None)
    range_reduce(C, thc[:], tf[:], ti[:], rr_[:])
    P.act(cs[:], rr_[:], AF.Sin)
    are, aim = pt("are"), pt("aim")
    P.tt(are[:], mag[:], cs[:], ALU.mult)
    P.tt(aim[:], mag[:], sn[:], ALU.mult)
    den, am1, t1, t2, kre, kim = pt("den"), pt("am1"), pt("t1"), pt("t2"), pt("kre"), pt("kim")
    P.tt(den[:], lr[:], lr[:], ALU.mult)
    P.tt(t1[:], li[:], li[:], ALU.mult)
    P.tt(den[:], den[:], t1[:], ALU.add)
    P.recip(den[:], den[:])
    P.ts(am1[:], are[:], -1.0, ALU.add, None)
    P.tt(t1[:], am1[:], lr[:], ALU.mult)
    P.tt(t2[:], aim[:], li[:], ALU.mult)
    P.tt(t1[:], t1[:], t2[:], ALU.add)
    P.tt(kre[:], t1[:], den[:], ALU.mult)
    P.tt(t1[:], aim[:], lr[:], ALU.mult)
    P.tt(t2[:], am1[:], li[:], ALU.mult)
    P.tt(t1[:], t1[:], t2[:], ALU.subtract)
    P.tt(kim[:], t1[:], den[:], ALU.mult)
    cT, sT, nsT, thT = pt("cT"), pt("sT"), pt("nsT"), pt("thT")
    P.ts(thT[:], th[:], 128.0, ALU.mult, None)
    range_reduce(C, thT[:], tf[:], ti[:], rr_[:])
    P.act(sT[:], rr_[:], AF.Sin)
    P.ts(thc[:], thT[:], math.pi / 2, ALU.add, None)
    range_reduce(C, thc[:], tf[:], ti[:], rr_[:])
    P.act(cT[:], rr_[:], AF.Sin)
    P.ts(nsT[:], sT[:], -1.0, ALU.mult, None)
    cosT = P.sbuf("cosT", [128, 16, 512], F32)
    sinT = P.sbuf("sinT", [128, 16, 512], F32)
    c512, s512, ns512 = pt("c512"), pt("s512"), pt("ns512")
    bbre = P.sbuf("bbre", [128, 16, 16], BF16)
    bbim = P.sbuf("bbim", [128, 16, 16], BF16)
    Bpad = P.sbuf("Bpad", [128, 4, 8, 2, 64], BF16)
    Cpad = P.sbuf("Cpad", [128, 16, 2, 128], BF16)
    dcol = P.sbuf("dcol", [128, 4], F32)
    Dg = P.sbuf("Dg", [128, 4, 128], BF16)
    gw = P.sbuf("gw", [128, 4, 512], BF16)
    gbc = P.sbuf("gbc", [128, 4], F32)
    mtmp = P.mark()
    mtab = P.mark()
    pos = P.sbuf("pos", [128, 128], F32)
    P.dma(pos[:], I["c_pos"][:, 0:128])
    ang = P.sbuf("ang", [128, 16, 128], F32)
    tfb = P.sbuf("tfb", [128, 16, 128], F32)
    tib = P.sbuf("tib", [128, 16, 128], I32)
    rrb = P.sbuf("rrb", [128, 16, 128], F32)
    P.tt(ang[:], W(th[:], th.h[:, :].unsqueeze(2).to_broadcast([128, 16, 128])),
         W(pos[:], pos.h[:, :].unsqueeze(1).to_broadcast([128, 16, 128])), ALU.mult)
    range_reduce(C, ang[:], tfb[:], tib[:], rrb[:])
    P.act(sinT[:, :, 0:128], rrb[:], AF.Sin)
    P.ts(ang[:], ang[:], math.pi / 2, ALU.add, None)
    range_reduce(C, ang[:], tfb[:], tib[:], rrb[:])
    P.act(cosT[:, :, 0:128], rrb[:], AF.Sin)
    c256, s256, tq = pt("c256"), pt("s256"), pt("tq")
    P.tt(c256[:], cT[:], cT[:], ALU.mult)
    P.tt(tq[:], sT[:], sT[:], ALU.mult)
    P.tt(c256[:], c256[:], tq[:], ALU.subtract)
    P.tt(s256[:], cT[:], sT[:], ALU.mult)
    P.ts(s256[:], s256[:], 2.0, ALU.mult, None)
    P.tt(c512[:], c256[:], c256[:], ALU.mult)
    P.tt(tq[:], s256[:], s256[:], ALU.mult)
    P.tt(c512[:], c512[:], tq[:], ALU.subtract)
    P.tt(s512[:], c256[:], s256[:], ALU.mult)
    P.ts(s512[:], s512[:], 2.0, ALU.mult, None)
    P.ts(ns512[:], s512[:], -1.0, ALU.mult, None)
    e1, e2 = ang, tfb
    for (src0, dst0, cc, ss) in ((0, 128, cT, sT), (0, 256, c256, s256), (128, 384, c256, s256)):
        cb_ = W(cc[:], cc.h[:, :].unsqueeze(2).to_broadcast([128, 16, 128]))
        sb_ = W(ss[:], ss.h[:, :].unsqueeze(2).to_broadcast([128, 16, 128]))
        cs_ = cosT[:, :, src0:src0 + 128]
        sn_ = sinT[:, :, src0:src0 + 128]
        P.tt(e1[:], cs_, cb_, ALU.mult)
        P.tt(e2[:], sn_, sb_, ALU.mult, eng="pool")
        P.tt(cosT[:, :, dst0:dst0 + 128], e1[:], e2[:], ALU.subtract)
        P.tt(e1[:], sn_, cb_, ALU.mult)
        P.tt(e2[:], cs_, sb_, ALU.mult, eng="pool")
        P.tt(sinT[:, :, dst0:dst0 + 128], e1[:], e2[:], ALU.add)
    P.release(mtab)
    bre = P.sbuf("bre", [128, 16, 16], F32)
    bim = P.sbuf("bim", [128, 16, 16], F32)
    for h in range(2):
        P.dma(bre[h * 64:(h + 1) * 64, :, :],
              V(I["s5_b_re"].h[0].rearrange("(G h) p m -> h p G m", h=2)[h], [("s5_b_re",)]))
        P.dma(bim[h * 64:(h + 1) * 64, :, :],
              V(I["s5_b_im"].h[0].rearrange("(G h) p m -> h p G m", h=2)[h], [("s5_b_im",)]))
    kre_b = W(kre[:], kre.h[:, :].unsqueeze(2).to_broadcast([128, 16, 16]))
    kim_b = W(kim[:], kim.h[:, :].unsqueeze(2).to_broadcast([128, 16, 16]))
    u1 = P.sbuf("u1", [128, 16, 16], F32)
    u2 = P.sbuf("u2", [128, 16, 16], F32)
    P.tt(u1[:], bre[:], kre_b, ALU.mult)
    P.tt(u2[:], bim[:], kim_b, ALU.mult)
    P.tt(bbre[:], u1[:], u2[:], ALU.subtract)
    P.tt(u1[:], bim[:], kre_b, ALU.mult)
    P.tt(u2[:], bre[:], kim_b, ALU.mult)
    P.tt(bbim[:], u1[:], u2[:], ALU.add)
    gmask = P.sbuf("gmask", [128, 8], F32)
    P.dma(gmask[:], I["c_gmask"][:])
    ptb = [P.psum("ptb", [128, 64], BF16) for _ in range(2)]
    n = 0
    for T_ in range(4):
        for ri, bb in enumerate((bbre, bbim)):
            pp = ptb[n % 2]
            n += 1
            for h in range(2):
                src = W(bb[:], bb.h[h * 64:(h + 1) * 64, T_ * 4:(T_ + 1) * 4, :].rearrange("p g m -> p (g m)"))
                P.transpose(pp[h * 64:(h + 1) * 64, :], src, C.identb[h * 64:(h + 1) * 64, h * 64:(h + 1) * 64])
            for g8 in range(8):
                P.ts(Bpad[:, T_, g8, ri, :], pp[:], gmask[:, g8:g8 + 1], ALU.mult, None)
    cN = P.sbuf("cN", [128, 4, 2, 64], F32)
    for ri, nm in enumerate(("s5_c_re", "s5_c_im")):
        for T_ in range(4):
            for h in range(2):
                for gl in range(4):
                    g = (T_ * 4 + gl) * 2 + h
                    P.dma(cN[h * 64 + gl * 16:h * 64 + gl * 16 + 16, T_, ri, :], V(I[nm].h[0, g], [(nm,)]))
    cNb = P.sbuf("cNb", [128, 4, 2, 64], BF16)
    P.copy(cNb[:], cN[:])
    P.memset(Cpad[:], 0.0)
    ptc = [P.psum("ptc", [128, 128], BF16) for _ in range(2)]
    n = 0
    for T_ in range(4):
        for ri in range(2):
            pp = ptc[n % 2]
            n += 1
            for h in range(2):
                P.transpose(pp[h * 64:(h + 1) * 64, :], cNb[:, T_, ri, :], C.identb[:])
            for h in range(2):
                for gl in range(4):
                    G = T_ * 4 + gl
                    c0 = h * 64 + gl * 16
                    if ri == 0:
                        P.copy(Cpad[h * 64:(h + 1) * 64, G, 0, c0:c0 + 16], pp[h * 64:(h + 1) * 64, c0:c0 + 16])
                    else:
                        P.ts(Cpad[h * 64:(h + 1) * 64, G, 1, c0:c0 + 16], pp[h * 64:(h + 1) * 64, c0:c0 + 16],
                             -1.0, ALU.mult, None)
    for h in range(2):
        for gl in range(4):
            src = I["s5_d"].h[0].rearrange("(T gl h) m -> gl h m T", gl=4, h=2)[gl, h]
            P.dma(dcol[h * 64 + gl * 16:h * 64 + gl * 16 + 16, :], V(src, [("s5_d",)]), allow_slow_non_contiguous=True)
    for T_ in range(4):
        P.ts(Dg[:, T_, :], C.identf[:], dcol[:, T_:T_ + 1], ALU.mult, None)
    Wg_ = dram2d(I["s5_glu_w"], 0)
    gwf = P.sbuf("gwf", [128, 4, 512], F32)
    for Ti in range(4):
        for h in range(2):
            for gl in range(4):
                r0 = Ti * 128 + gl * 32 + h * 16
                P.dma(gwf[h * 64 + gl * 16:h * 64 + gl * 16 + 16, Ti, :], V(Wg_.h[r0:r0 + 16, :], [(Wg_.name,)]))
    for Ti in range(4):
        for T_ in range(4):
            for h2 in range(2):
                dst = W(gw[:], gw.h[:, Ti, T_ * 128 + h2 * 64:T_ * 128 + (h2 + 1) * 64].rearrange("p (gl m) -> p gl m", gl=4))
                srcv = W(gwf[:], gwf.h[:, Ti, T_ * 128:(T_ + 1) * 128].rearrange("p (gl h m) -> p gl h m", gl=4, h=2)[:, :, h2, :])
                P.copy(dst, srcv, eng=("dve" if (T_ + h2) % 2 == 0 else "pool"))
    for h in range(2):
        for gl in range(4):
            src = I["s5_glu_b"].h[0].rearrange("(T gl h m) -> gl h m T", T=4, gl=4, h=2)[gl, h]
            P.dma(gbc[h * 64 + gl * 16:h * 64 + gl * 16 + 16, :], V(src, [("s5_glu_b",)]), allow_slow_non_contiguous=True)
    if "s5setup" in C.tap_req:
        for nm, src, shp, dt_ in (("cosT", cosT, [128, 16, 128], F32), ("sinT", sinT, [128, 16, 128], F32),
                                  ("are", are, [128, 16], F32), ("aim", aim, [128, 16], F32), ("kre", kre, [128, 16], F32),
                                  ("kim", kim, [128, 16], F32), ("cT", cT, [128, 16], F32), ("sT", sT, [128, 16], F32),
                                  ("Bpad", Bpad, [128, 4, 8, 2, 64], BF16), ("Cpad", Cpad, [128, 16, 2, 128], BF16),
                                  ("Dg", Dg, [128, 4, 128], BF16), ("gw", gw, [128, 4, 512], BF16), ("gbc", gbc, [128, 4], F32)):
            t3 = P.dram("tap_" + nm, shp, dt_, kind="ExternalOutput")
            P.dma(t3[:], src[:])
        P.flush(barrier=True)
    P.release(mtmp)
    print("sbuf remaining before s5 main", C.nc.sbuf_bytes_remaining)
    if C.stop_after == "s5setup":
        P.release(m)
        return
    psZ = [P.psum("psZ", [128, 2, 512], F32) for _ in range(2)]
    psY = [P.psum("psY", [128, 512], F32) for _ in range(2)]
    psG = [P.psum("psG", [128, 512], F32) for _ in range(2)]
    wre = [P.sbuf("wre", [128, 512], F32) for _ in range(2)]
    wim = [P.sbuf("wim", [128, 512], F32) for _ in range(2)]
    tb = [P.sbuf("tb", [128, 512], F32) for _ in range(2)]
    xpr = [P.sbuf("xpr", [128, 512], F32) for _ in range(2)]
    xpi = [P.sbuf("xpi", [128, 512], F32) for _ in range(2)]
    xre = [P.sbuf("xre", [128, 512], BF16) for _ in range(2)]
    xim = [P.sbuf("xim", [128, 512], BF16) for _ in range(2)]
    init = P.sbuf("init", [128, 16, 2], F32, nsub=16)
    tc_ = P.sbuf("tc", [128, 16, 2], F32, nsub=16)
    P.memset(init[:], 0.0)
    zb = [P.sbuf("zb", [128, 4, 512], BF16) for _ in range(1)]
    yst = [P.sbuf("yst", [128, 4, 512], BF16) for _ in range(1)]
    t4s = [P.sbuf("t4s", [128, 512], F32) for _ in range(2)]

    def v4(v, ap):
        return W(v, ap.rearrange("p (c t) -> p c t", c=4))

    def s5_iter(b, T_, gl, i2, py):
        G = T_ * 4 + gl
        pz_ = psZ[i2]
        for h in range(2):
            g8 = h * 4 + gl
            for ri in range(2):
                P.mm(pz_[h * 64:(h + 1) * 64, ri, :], Bpad[:, T_, g8, ri, :], uT.s(b)[:, T_, b * 512:(b + 1) * 512])
        yield
        cos_b = cosT[:, G, :]
        sin_b = sinT[:, G, :]
        zre4 = pz_[:, 0, :]
        zim4 = pz_[:, 1, :]
        wr_, wi_ = wre[i2], wim[i2]
        t1, t2, t3, t4 = wr_, tb[i2], wi_, t4s[i2]
        P.tt(t1[:], zre4, cos_b, ALU.mult)
        yield
        P.tt(t2[:], zim4, sin_b, ALU.mult)
        yield
        P.tt(t3[:], zim4, cos_b, ALU.mult)
        yield
        P.tt(t4[:], zre4, sin_b, ALU.mult)
        yield
        P.tt(wr_[:], t1[:], t2[:], ALU.add, eng="pool")
        yield
        P.tt(wi_[:], t3[:], t4[:], ALU.subtract, eng="pool")
        yield
        xr_, xi_ = xpr[i2], xpi[i2]
        rho_b = W(mag[:], mag.h[:, G:G + 1].to_broadcast([128, 512]))
        P.scan(xr_[:], rho_b, wr_[:], init.s(G)[:, G, 0:1])
        yield
        P.scan(xi_[:], rho_b, wi_[:], init.s(G)[:, G, 1:2])
        yield
        er = xr_[:, 511:512]
        ei = xi_[:, 511:512]
        P.act(tc_.s(G)[:, G, 0:1], er, AF.Copy, scale=c512[:, G:G + 1])
        yield
        P.act(tc_.s(G)[:, G, 1:2], er, AF.Copy, scale=s512[:, G:G + 1])
        yield
        P.act(init.s(G)[:, G, 0:1], ei, AF.Identity, scale=ns512[:, G:G + 1], bias=tc_.s(G)[:, G, 0:1])
        yield
        P.act(init.s(G)[:, G, 1:2], ei, AF.Identity, scale=c512[:, G:G + 1], bias=tc_.s(G)[:, G, 1:2])
        yield
        P.tt(t1[:], xr_[:], cos_b, ALU.mult)
        yield
        P.tt(t2[:], xi_[:], sin_b, ALU.mult)
        yield
        P.tt(t3[:], xr_[:], sin_b, ALU.mult, eng="pool")
        yield
        P.tt(t4[:], xi_[:], cos_b, ALU.mult, eng="pool")
        yield
        P.tt(xre[i2][:], t1[:], t2[:], ALU.subtract)
        yield
        P.tt(xim[i2][:], t3[:], t4[:], ALU.add, eng="pool")
        yield
        P.mm(py[:], Cpad[:, G, 0, :], xre[i2][:], start=(gl == 0), stop=False)
        P.mm(py[:], Cpad[:, G, 1, :], xim[i2][:], start=False, stop=False)
        yield

    def interleave(gens):
        gens = list(gens)
        while gens:
            nxt = []
            for g in gens:
                try:
                    next(g)
                    nxt.append(g)
                except StopIteration:
                    pass
            gens = nxt

    ny = 0
    for b in range(4):
        zb_ = zb[0]
        yst_ = yst[0]
        for T_ in range(4):
            py = psY[ny % 2]
            ny += 1
            for gp in range(2):
                interleave([s5_iter(b, T_, gp * 2 + 0, 0, py), s5_iter(b, T_, gp * 2 + 1, 1, py)])
            P.mm(py[:], Dg[:, T_, :], uT.s(b)[:, T_, b * 512:(b + 1) * 512], start=False, stop=True)
            P.act(zb_[:, T_, :], py[:], AF.Gelu)
        for To in range(4):
            pg = psG[To % 2]
            s_ = tb[To % 2]
            for Ti in range(4):
                P.mm(pg[:], gw[:, Ti, To * 128:(To + 1) * 128], zb_[:, Ti, :], start=(Ti == 0), stop=(Ti == 3))
            P.act(s_[:], pg[:], AF.Sigmoid, bias=gbc[:, To:To + 1])
            P.tt(yst_[:, To, :], zb_[:, To, :], s_[:], ALU.mult)
        P.dma(V(mixT.h[0:512, b * 512:(b + 1) * 512].rearrange("(k p) t -> p k t", p=128), [(mixT.name, b)]), yst_[:])
    P.release(m)


def ssd_stage(C):
    P, I = C.P, C.I
    mixT, dtda = C.mixT, C.dtda
    m = P.mark()
    tri = P.sbuf("tri", [128, 128], F32)
    up = P.sbuf("up", [128, 128], F32)
    onesf = P.sbuf("onesf", [128, 128], F32)
    Dt = P.sbuf("Dt", [128, 16], F32)
    ng = P.sbuf("ng", [128, 1024], F32)
    P.dma(tri[:], I["c_tri"][:])
    P.dma(up[:], I["c_up"][:])
    P.memset(onesf[:], 1.0)
    P.dma(Dt[:], V(I["ssd_d"].h[0].partition_broadcast(128), [("ssd_d",)]))
    P.dma(ng[:], V(I["ssd_norm_g"].h[0].partition_broadcast(128), [("ssd_norm_g",)]))
    state = P.sbuf("state", [64, 16, 64], F32)
    state_bf = P.sbuf("state_bf", [64, 16, 64], BF16)
    P.memset(state[:], 0.0)
    P.memset(state_bf[:], 0.0)
    xa_ = [P.sbuf("sxa", [128, 1024], BF16) for _ in range(2)]
    xb_ = [P.sbuf("sxb", [128, 1024], BF16) for _ in range(2)]
    Bc_ = [P.sbuf("sB", [128, 256], BF16) for _ in range(2)]
    zc_ = [P.sbuf("sz", [128, 1024], BF16) for _ in range(2)]
    BTc_ = [P.sbuf("sBT", [64, 4, 128], BF16) for _ in range(4)]
    CTc_ = [P.sbuf("sCT", [64, 4, 128], BF16) for _ in range(4)]
    R_ = [P.sbuf("R", [128, 16, 128], F32) for _ in range(1)]
    seg_ = [P.sbuf("seg", [128, 16, 128], F32) for _ in range(1)]
    sc_ = [P.sbuf("scT", [128, 16, 128], BF16) for _ in range(1)]
    CBm_ = [P.sbuf("CBm", [128, 4, 128], F32) for _ in range(1)]
    ec_ = [P.sbuf("ec", [128, 2, 16], F32) for _ in range(3)]
    xdt_ = [P.sbuf("xdt", [128, 1024], BF16) for _ in range(3)]
    xdd_ = [P.sbuf("xdd", [128, 1024], BF16) for _ in range(2)]
    Y = P.sbuf("Y", [128, 1024], F32)
    Tt = P.sbuf("Tt", [128, 1024], F32)
    junk = P.sbuf("junk", [128, 1024], BF16)
    Yn_ = [P.sbuf("Yn", [128, 1024], BF16) for _ in range(2)]
    ss = P.sbuf("ss", [128, 2], F32)
    ybT = [P.sbuf("ybT", [128, 8, 512], BF16) for _ in range(1)]
    pd = P.psum("pd", [128, 2, 512], F32)
    pY = P.psum("pY", [128, 4, 512], F32)
    pcb = P.psum("pcb", [128, 4, 128], F32)
    pmix = P.psum("pmix", [128, 512], F32)
    ptr = W(pmix[:], pmix.h[:, 0:256].bitcast(BF16).rearrange("p (k t) -> p k t", k=4))
    psm0 = W(pmix[:], pmix.h[:, 256:272])
    psm1 = W(pmix[:], pmix.h[:, 272:288])
    psm = W(pmix[:], pmix.h[:, 256:288].rearrange("p (a b) -> p a b", a=2))

    def hd(v, ap):
        return W(v, ap.rearrange("p (h d) -> p h d", h=16))

    def s0(c):
        b = c // 4
        tok = slice(c * 128, (c + 1) * 128)
        P.dma(xa_[c % 2][:], C.d_x.s(b)[tok, :])
        P.dma(BTc_[c % 4][:], V(C.d_BT.h[:, tok].rearrange("(g n) t -> n g t", n=64), [(C.d_BT.name, b)]))
        P.dma(CTc_[c % 4][:], V(C.d_CT.h[:, tok].rearrange("(g n) t -> n g t", n=64), [(C.d_CT.name, b)]))

    def s1(c):
        R = R_[0]
        da_v = dtda.s(c)[:, c, 16:32]
        dt_v = dtda.s(c)[:, c, 0:16]
        P.tt(R[:], W(tri[:], tri.h[:, :].unsqueeze(1).to_broadcast([128, 16, 128])),
             W(da_v, dtda.h[:, c, 16:32].unsqueeze(2).to_broadcast([128, 16, 128])), ALU.mult)
        x_c = xa_[c % 2]
        xdt = xdt_[c % 3]
        P.tt(hd(xdt[:], xdt.h[:, :]), hd(x_c[:], x_c.h[:, :]),
             W(dt_v, dtda.h[:, c, 0:16].unsqueeze(2).to_broadcast([128, 16, 64])), ALU.mult, eng="pool")

    def s2(c):
        R = R_[0]
        seg = seg_[0]
        ec = ec_[c % 3]
        CBm = CBm_[0]
        da_v = dtda.s(c)[:, c, 16:32]
        BT_c, CT_c = BTc_[c % 4], CTc_[c % 4]
        P.mm(psm0, tri[:], da_v)
        P.mm(psm1, onesf[:], da_v)
        for g in range(4):
            P.mm(pcb[:, g, :], BT_c[:, g, :], CT_c[:, g, :])
        for half in range(2):
            for q in range(2):
                hq = half * 2 + q
                P.mm(pd[:, q, :], up[:], W(R[:], R.h[:, hq * 4:(hq + 1) * 4, :].rearrange("p h i -> p (h i)")))
            if half == 0:
                P.act(ec[:], psm, AF.Exp)
            P.act(W(seg[:], seg.h[:, half * 8:(half + 1) * 8, :].rearrange("p h i -> p (h i)")),
                  W(pd[:], pd.h[:, :, :].rearrange("p q i -> p (q i)")), AF.Exp)
        P.tt(CBm[:], pcb[:], W(tri[:], tri.h[:, :].unsqueeze(1).to_broadcast([128, 4, 128])), ALU.mult)

    def s3(c):
        seg = seg_[0]
        sc = sc_[0]
        CBm = CBm_[0]
        xdt = xdt_[c % 3]
        xdd = xdd_[c % 2]
        b_ = c // 4
        tok = slice(c * 128, (c + 1) * 128)
        P.dma(xb_[c % 2][:], C.d_x.s(b_)[tok, :])
        P.dma(Bc_[c % 2][:], C.d_B.s(b_)[tok, :])
        P.dma(zc_[c % 2][:], C.d_zs.s(c)[tok, :])
        P.tt(W(sc[:], sc.h[:, :, :].rearrange("p (g h) i -> p g h i", g=4)),
             W(seg[:], seg.h[:, :, :].rearrange("p (g h) i -> p g h i", g=4)),
             W(CBm[:], CBm.h[:, :, :].unsqueeze(2).to_broadcast([128, 4, 4, 128])), ALU.mult)
        P.tt(hd(xdd[:], xdd.h[:, :]), hd(xdt[:], xdt.h[:, :]),
             W(seg[:], seg.h[:, :, 127:128].to_broadcast([128, 16, 64])), ALU.mult, eng="pool")

    def s4(c):
        sc = sc_[0]
        xdt = xdt_[c % 3]
        CT_c = CTc_[c % 4]
        for h in range(16):
            P.mm(pY[:, h // 8, (h % 8) * 64:(h % 8 + 1) * 64], sc[:, h, :], xdt[:, h * 64:(h + 1) * 64])
        for g in range(4):
            P.mm(pY[:, 2 + g // 2, (g % 2) * 256:(g % 2 + 1) * 256], CT_c[:, g, :],
                 W(state_bf[:], state_bf.h[:, g * 4:(g + 1) * 4, :].rearrange("p h d -> p (h d)")))

    def s5(c):
        ec = ec_[c % 3]
        x_c, z_c, B_c = xb_[c % 2], zc_[c % 2], Bc_[c % 2]
        xdd = xdd_[c % 2]
        Yn = Yn_[c % 2]
        yi = W(pY[:], pY.h[:, 2:4, :].rearrange("p q (h d) -> p (q h) d", d=64))
        ya_ = W(pY[:], pY.h[:, 0:2, :].rearrange("p q i -> p (q i)"))
        P.tt(hd(Y[:], Y.h[:, :]), yi, W(ec[:], ec.h[:, 0, :].unsqueeze(2).to_broadcast([128, 16, 64])), ALU.mult)
        P.tt(Y[:], ya_, Y[:], ALU.add)
        for g in range(4):
            P.mm(pY[0:64, g // 2, (g % 2) * 256:(g % 2 + 1) * 256], B_c[:, g * 64:(g + 1) * 64], xdd[:, g * 256:(g + 1) * 256])
        P.tt(state[:], state[:], W(ec[:], ec.h[0:64, 1, :].unsqueeze(2).to_broadcast([64, 16, 64])), ALU.mult)
        P.tt(state[:], state[:], W(pY[:], pY.h[0:64, 0:2, :].rearrange("p q (h d) -> p (q h) d", d=64)), ALU.add)
        P.copy(state_bf[:], state[:], eng="act")
        P.tt(hd(Tt[:], Tt.h[:, :]), hd(x_c[:], x_c.h[:, :]),
             W(Dt[:], Dt.h[:, :].unsqueeze(2).to_broadcast([128, 16, 64])), ALU.mult, eng="pool")
        P.tt(Y[:], Y[:], Tt[:], ALU.add, eng="pool")
        P.tt(Y[:], Y[:], z_c[:], ALU.mult, eng="pool")
        P.act(junk[:], Y[:], AF.Square, accum_out=ss[:, 0:1])
        P.act(ss[:, 1:2], ss[:, 0:1], AF.Sqrt, bias=EPS, scale=1.0 / 1024)
        P.recip(ss[:, 1:2], ss[:, 1:2])
        P.stt(Yn[:], Y[:], ss[:, 1:2], ng[:], ALU.mult, ALU.mult)

    def s6(c):
        b = c // 4
        Yn = Yn_[c % 2]
        yb_ = ybT[0]
        for half in range(2):
            for k in range(4):
                kk = half * 4 + k
                P.transpose(W(ptr, ptr.ap[:, k, :]), Yn[:, kk * 128:(kk + 1) * 128], C.identb[:])
            P.copy(yb_[:, half * 4:(half + 1) * 4, (c % 4) * 128:(c % 4 + 1) * 128], ptr, eng="act")
        if c % 4 == 3:
            P.dma(V(mixT.h[512:1536, b * 512:(b + 1) * 512].rearrange("(k p) t -> p k t", p=128), [(mixT.name, b)]), yb_[:])

    pipeline(NT, [s0, s1, s2, s3, s4, s5, s6])
    P.release(m)


LN_G = [math.log(1.0 - 2.0 ** (-5.0 - h)) for h in range(4)]
DK = 128
QSC = DK ** -0.5


def layer1_mixer(C):
    P, I = C.P, C.I
    m0 = P.mark()
    Win = dram2d(I["od_in_w"], 0)
    d_QsT = P.dram("d_QsT", [1024, L], BF16, nsub=4)
    d_KsT = P.dram("d_KsT", [1024, L], BF16, nsub=4)
    d_Kst = P.dram("d_Kst", [L, 1024], BF16, nsub=4)
    d_V = P.dram("d_V", [L, 2048], BF16, nsub=NT)
    d_G = P.dram("d_G", [L, 2048], BF16, nsub=NT)
    mixT = P.dram("d_mixT1", [2048, L], BF16, nsub=4)
    C.mixT = mixT
    eblast = P.sbuf("eblast", [128, NT, 4], F32)
    tri = P.sbuf("tri1", [128, 128], F32)
    P.dma(tri[:], I["c_tri"][:])

    mB = P.mark()
    hnT = P.sbuf("hnT1", [128, KT, L], BF16, nsub=4)
    m2 = P.mark()
    norm_T(C, 1, hnT)
    P.release(m2)

    cosF = P.sbuf("cosF", [128, L], F32)
    sinS = P.sbuf("sinS", [128, L], F32)
    Eq = P.sbuf("Eq", [128, 4, 128], F32)
    Ek = P.sbuf("Ek", [128, 4, 128], F32)
    Es = P.sbuf("Es", [128, 4, 128], F32)
    m2 = P.mark()
    pos = P.sbuf("posf", [128, L], F32)
    P.dma(pos[:], I["c_pos"][:])
    dmod = P.sbuf("dmod", [128, 1], F32)
    sgn = P.sbuf("sgn", [128, 1], F32)
    invf = P.sbuf("invf", [128, 1], F32)
    P.dma(dmod[:], I["c_dmod"][:])
    P.dma(sgn[:], I["c_sign"][:])
    P.act(invf[:], dmod[:], AF.Exp, scale=-math.log(10000.0) / 64.0)
    ang = P.sbuf("angf", [128, L], F32)
    tfb = P.sbuf("tfbf", [128, L], F32)
    tib = P.sbuf("tibf", [128, L], I32)
    rrb = P.sbuf("rrbf", [128, L], F32)
    P.ts(ang[:], pos[:], invf[:, 0:1], ALU.mult, None)
    range_reduce(C, ang[:], tfb[:], tib[:], rrb[:])
    P.act(sinS[:], rrb[:], AF.Sin)
    P.ts(sinS[:], sinS[:], sgn[:, 0:1], ALU.mult, None)
    P.ts(ang[:], ang[:], math.pi / 2, ALU.add, None)
    range_reduce(C, ang[:], tfb[:], tib[:], rrb[:])
    P.act(cosF[:], rrb[:], AF.Sin)
    for h in range(4):
        lg = LN_G[h]
        P.act(Eq[:, h, :], pos[:, 0:128], AF.Exp, scale=lg, bias=lg)
        P.act(Ek[:, h, :], pos[:, 0:128], AF.Exp, scale=-lg, bias=-lg + math.log(QSC))
        P.act(Es[:, h, :], pos[:, 0:128], AF.Exp, scale=-lg, bias=127.0 * lg + math.log(QSC))
    P.release(m2)

    m2 = P.mark()
    wn = [P.sbuf("wn", [128, KT, 128], BF16) for _ in range(2)]
    ws = [P.sbuf("wsw", [128, KT, 128], BF16) for _ in range(2)]
    pn = [P.psum("pn", [128, 512], F32) for _ in range(2)]
    psw = [P.psum("psw", [128, 512], F32) for _ in range(2)]
    ptk = [P.psum("ptk", [128, 4, 128], BF16) for _ in range(2)]
    r1 = [P.sbuf("r1", [128, 512], F32) for _ in range(2)]
    r2 = [P.sbuf("r2", [128, 512], F32) for _ in range(2)]
    ob = [P.sbuf("ob", [128, 512], BF16) for _ in range(4)]
    kst = [P.sbuf("kst", [128, 512], BF16) for _ in range(2)]
    kstg = [P.sbuf("kstg", [128, 4, 128], BF16) for _ in range(2)]
    def e4(tb_, h):
        return W(tb_[:], tb_.h[:, h, :].unsqueeze(1).to_broadcast([128, 4, 128]))

    def o4(o_):
        return W(o_[:], o_.h[:, :].rearrange("p (c t) -> p c t", c=4))

    def f0(it):
        wh, b = it // 4, it % 4
        which, h = wh // 4, wh % 4
        w_ = wn[wh % 2]
        s_ = ws[wh % 2]
        if b == 0:
            load_w(C, w_[:], Win, which * 512 + h * 128, 128)
            P.copy(s_[:, :, 0:64], w_[:, :, 64:128], eng="pool")
            P.copy(s_[:, :, 64:128], w_[:, :, 0:64], eng="pool")
        i2 = it % 2
        blk = slice(b * 512, (b + 1) * 512)
        for k in range(KT):
            P.mm(pn[i2][:], w_[:, k, :], hnT.s(b)[:, k, blk], start=(k == 0), stop=(k == KT - 1))
        for k in range(KT):
            P.mm(psw[i2][:], s_[:, k, :], hnT.s(b)[:, k, blk], start=(k == 0), stop=(k == KT - 1))

    def f1(it):
        b = it % 4
        i2 = it % 2
        blk = slice(b * 512, (b + 1) * 512)
        P.tt(r1[i2][:], pn[i2][:], cosF[:, blk], ALU.mult)
        P.tt(r2[i2][:], psw[i2][:], sinS[:, blk], ALU.mult)
        P.tt(r1[i2][:], r1[i2][:], r2[i2][:], ALU.add, eng="pool")

    def f2(it):
        wh, b = it // 4, it % 4
        which, h = wh // 4, wh % 4
        i2 = it % 2
        blk = slice(b * 512, (b + 1) * 512)
        r4 = W(r1[i2][:], r1[i2].h[:, :].rearrange("p (c t) -> p c t", c=4))
        o_ = ob[it % 4]
        if which == 0:
            P.tt(o4(o_), r4, e4(Eq, h), ALU.mult)
            P.dma(d_QsT.s(b)[h * 128:(h + 1) * 128, blk], o_[:])
        else:
            P.tt(o4(o_), r4, e4(Ek, h), ALU.mult)
            P.dma(d_KsT.s(b)[h * 128:(h + 1) * 128, blk], o_[:])
            P.tt(o4(kst[i2]), r4, e4(Es, h), ALU.mult, eng="pool")

    def f3(it):
        wh, b = it // 4, it % 4
        which, h = wh // 4, wh % 4
        if which == 0:
            return
        i2 = it % 2
        blk = slice(b * 512, (b + 1) * 512)
        k_ = kst[i2]
        for j in range(4):
            P.transpose(ptk[i2][:, j, :], k_[:, j * 128:(j + 1) * 128], C.identb[:])
        P.copy(kstg[i2][:], ptk[i2][:], eng="act")
        P.dma(V(d_Kst.h[blk, h * 128:(h + 1) * 128].rearrange("(j p) c -> p j c", p=128), [(d_Kst.name, b)]),
              kstg[i2][:])

    pipeline(32, [f0, f1, f2, f3])
    P.release(m2)

    m2 = P.mark()
    walr = P.sbuf("walr", [128, KT, 16], BF16)
    load_w(C, walr[:], Win, 5120, 16)
    alrT = P.sbuf("alrT", [16, L], F32)
    gwt = P.sbuf("gwt", [16, 512], F32)
    P.dma(gwt[:], I["gla_gate_w"][0])
    nb = P.sbuf("nb", [128, 4], F32)
    P.dma(nb[:], V(I["gla_gate_b"].h[0].rearrange("(h d) -> d h", d=128), [("gla_gate_b",)]), allow_slow_non_contiguous=True)
    P.ts(nb[:], nb[:], -1.0, ALU.mult, None)
    ones = P.sbuf("ones1", [128, 128], F32)
    P.memset(ones[:], 1.0)
    pa = [P.psum("pa", [16, 512], F32) for _ in range(2)]
    for b in range(4):
        blk = slice(b * 512, (b + 1) * 512)
        for k in range(KT):
            P.mm(pa[b % 2][:], walr[:, k, :], hnT.s(b)[:, k, blk], start=(k == 0), stop=(k == KT - 1))
        P.copy(alrT[:, blk], pa[b % 2][:])
    wq = [P.sbuf("wq", [128, KT, 128], BF16) for _ in range(2)]
    wk = [P.sbuf("wk", [128, KT, 128], BF16) for _ in range(2)]
    pl = P.psum("pl", [128, 512], F32)
    pq = P.psum("pq", [128, 512], F32)
    pk = P.psum("pk", [128, 512], F32)
    ptk = [P.psum("ptk2", [128, 4, 128], BF16) for _ in range(2)]
    la = [P.sbuf("la", [128, 512], F32) for _ in range(2)]
    bc = [P.sbuf("bc", [128, 512], F32) for _ in range(2)]
    eb = [P.sbuf("eb", [128, 512], F32) for _ in range(2)]
    enb = [P.sbuf("enb", [128, 512], F32) for _ in range(2)]
    est = [P.sbuf("est", [128, 512], F32) for _ in range(2)]
    ob = [P.sbuf("ob2", [128, 512], BF16) for _ in range(4)]
    kst = [P.sbuf("kst2", [128, 512], BF16) for _ in range(2)]
    kstg = [P.sbuf("kstg2", [128, 4, 128], BF16) for _ in range(2)]
    n = 0
    no = 0
    for h in range(4):
        q_ = wq[h % 2]
        k_w = wk[h % 2]
        load_w(C, q_[:], Win, 3072 + h * 128, 128)
        load_w(C, k_w[:], Win, 3584 + h * 128, 128)
        for b in range(4):
            i2 = n % 2
            n += 1
            blk = slice(b * 512, (b + 1) * 512)
            P.mm(pl[:], gwt[:, h * 128:(h + 1) * 128], alrT[:, blk])
            P.act(la[i2][:], pl[:], AF.Exp, scale=-1.0, bias=nb[:, h:h + 1])
            P.act(la[i2][:], la[i2][:], AF.Ln, bias=1.0)
            P.ts(la[i2][:], la[i2][:], -1.0 / 16.0, ALU.mult, None)
            for c in range(4):
                sl = slice(c * 128, (c + 1) * 128)
                P.scan(bc[i2][:, sl], ones[:], la[i2][:, sl], 0.0)
            P.act(eb[i2][:], bc[i2][:], AF.Exp)
            P.act(enb[i2][:], bc[i2][:], AF.Exp, scale=-1.0)
            for c in range(4):
                sl = slice(c * 128, (c + 1) * 128)
                P.act(est[i2][:, sl], bc[i2][:, sl], AF.Exp, scale=-1.0, bias=bc[i2][:, c * 128 + 127:c * 128 + 128])
            P.act(eblast[:, b * 4:(b + 1) * 4, h], W(bc[i2][:], bc[i2].h[:, :].rearrange("p (c t) -> p c t", c=4)[:, :, 127]), AF.Exp)
            for k in range(KT):
                P.mm(pq[:], q_[:, k, :], hnT.s(b)[:, k, blk], start=(k == 0), stop=(k == KT - 1))
            for k in range(KT):
                P.mm(pk[:], k_w[:, k, :], hnT.s(b)[:, k, blk], start=(k == 0), stop=(k == KT - 1))
            o_ = ob[no % 4]
            no += 1
            P.stt(o_[:], pq[:], QSC, eb[i2][:], ALU.mult, ALU.mult)
            P.dma(d_QsT.s(b)[(4 + h) * 128:(5 + h) * 128, blk], o_[:])
            o_ = ob[no % 4]
            no += 1
            P.tt(o_[:], pk[:], enb[i2][:], ALU.mult)
            P.dma(d_KsT.s(b)[(4 + h) * 128:(5 + h) * 128, blk], o_[:])
            k_ = kst[i2]
            P.tt(k_[:], pk[:], est[i2][:], ALU.mult)
            for j in range(4):
                P.transpose(ptk[i2][:, j, :], k_[:, j * 128:(j + 1) * 128], C.identb[:])
            P.copy(kstg[i2][:], ptk[i2][:], eng="act")
            P.dma(V(d_Kst.h[blk, (4 + h) * 128:(5 + h) * 128].rearrange("(j p) c -> p j c", p=128), [(d_Kst.name, b)]),
                  kstg[i2][:])
    P.release(m2)

    m2 = P.mark()
    ngt = P.sbuf("ngt", [128, 2048], F32)
    P.dma(ngt[:, 0:1024], V(I["ret_norm_g"].h[0].partition_broadcast(128), [("ret_norm_g",)]))
    P.dma(ngt[:, 1024:2048], V(I["gla_norm_g"].h[0].partition_broadcast(128), [("gla_norm_g",)]))
    wv = [P.sbuf("wv", [128, KT, 512], BF16) for _ in range(2)]
    pv = [P.psum("pv", [128, 512], F32) for _ in range(4)]
    vs = [P.sbuf("vs", [128, 512], BF16) for _ in range(3)]
    sgt = [P.sbuf("sgt", [128, 512], F32) for _ in range(2)]
    jobs = [(1024, 0, 0), (1536, 0, 512), (4096, 0, 1024), (4608, 0, 1536),
            (2048, 1, 0), (2560, 1, 512), (5136, 1, 1024), (5648, 1, 1536)]
    n = 0
    load_w(C, wv[0][:], Win, jobs[0][0], 512)
    for ji, (c0, isg, dc) in enumerate(jobs):
        w_ = wv[ji % 2]
        if ji + 1 < len(jobs):
            load_w(C, wv[(ji + 1) % 2][:], Win, jobs[ji + 1][0], 512)
        for t in range(NT):
            pp = pv[n % 4]
            o_ = vs[n % 3]
            s_ = sgt[n % 2]
            n += 1
            tok = slice(t * 128, (t + 1) * 128)
            for k in range(KT):
                P.mm(pp[:], hnT.s(t // 4)[:, k, tok], w_[:, k, :], start=(k == 0), stop=(k == KT - 1))
            if isg == 0:
                if n % 2 == 0:
                    P.copy(o_[:], pp[:], eng="act")
                else:
                    P.copy(o_[:], pp[:])
                P.dma(d_V.s(t)[tok, dc:dc + 512], o_[:])
            else:
                P.act(s_[:], pp[:], AF.Silu)
                P.tt(o_[:], s_[:], ngt[:, dc:dc + 512], ALU.mult)
                P.dma(d_G.s(t)[tok, dc:dc + 512], o_[:])
    P.release(m2)
    P.release(mB)

    wo1 = out_proj_load(C, dram2d(I["od_out_w"], 0), 16)
    mC = P.mark()
    state = [P.sbuf("st1", [128, 4, 256], F32) for _ in range(2)]
    state_bf = [P.sbuf("st1b", [128, 4, 256], BF16) for _ in range(2)]
    for mx in range(2):
        P.memset(state[mx][:], 0.0)
        P.memset(state_bf[mx][:], 0.0)
    NS = 3
    Qc = [P.sbuf("Qc", [128, 8, 128], BF16) for _ in range(NS)]
    Kc = [P.sbuf("Kc", [128, 8, 128], BF16) for _ in range(NS)]
    Ksc = [P.sbuf("Ksc", [128, 1024], BF16) for _ in range(NS)]
    Vc = [P.sbuf("Vc", [128, 2048], BF16) for _ in range(NS)]
    Gc = [P.sbuf("Gc", [128, 2048], BF16) for _ in range(NS)]
    att = [P.sbuf("att", [128, 4, 128], BF16) for _ in range(3)]
    oS = [P.sbuf("oS", [128, 4, 256], F32) for _ in range(3)]
    sq = [P.sbuf("sq1", [128, 4, 256], F32) for _ in range(2)]
    on = [P.sbuf("on1", [128, 4, 256], F32) for _ in range(2)]
    fin = [P.sbuf("fin1", [128, 1024], BF16) for _ in range(3)]
    st = [P.sbuf("stat", [128, 4, 4], F32) for _ in range(4)]
    stg = [P.sbuf("stg1", [128, 16, 512], BF16) for _ in range(1)]
    pat = [P.psum("pat", [128, 4, 128], F32) for _ in range(2)]
    pout = P.psum("pout", [128, 2, 512], F32)
    pstt = P.psum("pstt", [128, 2, 512], F32)
    ptr = P.psum("ptr1", [128, 8, 128], BF16)
    tri_b = W(tri[:], tri.h[:, :].unsqueeze(1).to_broadcast([128, 4, 128]))

    def ph0(it):
        c, mx = it // 2, it % 2
        if mx:
            return
        s_ = c % NS
        b = c // 4
        tok = slice(c * 128, (c + 1) * 128)
        P.dma(Qc[s_][:], V(d_QsT.h[:, tok].rearrange("(k p) t -> p k t", p=128), [(d_QsT.name, b)]))
        P.dma(Kc[s_][:], V(d_KsT.h[:, tok].rearrange("(k p) t -> p k t", p=128), [(d_KsT.name, b)]))
        P.dma(Ksc[s_][:], d_Kst.s(b)[tok, :])
        P.dma(Vc[s_][:], d_V.s(c)[tok, :])
        P.dma(Gc[s_][:], d_G.s(c)[tok, :])

    def ph1(it):
        c, mx = it // 2, it % 2
        s_ = c % NS
        for h in range(4):
            P.mm(pat[it % 2][:, h, :], Kc[s_][:, mx * 4 + h, :], Qc[s_][:, mx * 4 + h, :])
        P.tt(att[it % 3][:], pat[it % 2][:], tri_b, ALU.mult)

    def ph2(it):
        c, mx = it // 2, it % 2
        s_ = c % NS
        for h in range(4):
            ov = pout[:, h // 2, (h % 2) * 256:(h % 2 + 1) * 256]
            P.mm(ov, att[it % 3][:, h, :], Vc[s_][:, mx * 1024 + h * 256:mx * 1024 + (h + 1) * 256], start=True, stop=False)
            P.mm(ov, Qc[s_][:, mx * 4 + h, :], state_bf[mx][:, h, :], start=False, stop=True)
        for h in range(4):
            P.mm(pstt[:, h // 2, (h % 2) * 256:(h % 2 + 1) * 256], Ksc[s_][:, mx * 512 + h * 128:mx * 512 + (h + 1) * 128],
                 Vc[s_][:, mx * 1024 + h * 256:mx * 1024 + (h + 1) * 256])

    def ph3(it):
        c, mx = it // 2, it % 2
        o_ = oS[it % 3]
        P.copy(o_[:], W(pout[:], pout.h[:, :, :].rearrange("p q (h d) -> p (q h) d", d=256)), eng="act")
        for h in range(4):
            sp = pstt[:, h // 2, (h % 2) * 256:(h % 2 + 1) * 256]
            if mx == 0:
                P.stt(state[mx][:, h, :], state[mx][:, h, :], math.exp(LN_G[h] * 128.0), sp, ALU.mult, ALU.add)
            else:
                P.stt(state[mx][:, h, :], state[mx][:, h, :], eblast[:, c, h:h + 1], sp, ALU.mult, ALU.add)
        P.copy(state_bf[mx][:], state[mx][:], eng="act")

    def ph4(it):
        c, mx = it // 2, it % 2
        o_ = oS[it % 3]
        st_ = st[it % 4]
        sq_ = sq[it % 2]
        P.act(sq_[:], o_[:], AF.Square)
        P.add("dve", (lambda sq_=sq_, st_=st_: C.nc.vector.tensor_reduce(st_.h[:, :, 1], sq_.h[:, :, :], AX.X, ALU.add)), [sq_[:]], [st_[:]])
        if mx == 0:
            P.add("dve", (lambda o_=o_, st_=st_: C.nc.vector.tensor_reduce(st_.h[:, :, 0], o_.h[:, :, :], AX.X, ALU.add)), [o_[:]], [st_[:]])
            P.ts(st_[:, :, 0], st_[:, :, 0], 1.0 / 256, ALU.mult, None)
            P.tt(st_[:, :, 2], st_[:, :, 0], st_[:, :, 0], ALU.mult)
            P.stt(st_[:, :, 1], st_[:, :, 1], 1.0 / 256, st_[:, :, 2], ALU.mult, ALU.subtract)
        else:
            P.ts(st_[:, :, 1], st_[:, :, 1], 1.0 / 256, ALU.mult, None)
        P.act(st_[:, :, 3], st_[:, :, 1], AF.Sqrt, bias=EPS)
        P.recip(st_[:, :, 3], st_[:, :, 3])

    def ph5(it):
        c, mx = it // 2, it % 2
        s_ = c % NS
        o_ = oS[it % 3]
        st_ = st[it % 4]
        on_ = on[it % 2]
        fin_ = fin[it % 3]
        rb = W(st_[:], st_.h[:, :, 3:4].to_broadcast([128, 4, 256]))
        if mx == 0:
            mb_ = W(st_[:], st_.h[:, :, 0:1].to_broadcast([128, 4, 256]))
            P.tt(on_[:], o_[:], mb_, ALU.subtract, eng="pool")
            P.tt(on_[:], on_[:], rb, ALU.mult, eng="pool")
        else:
            P.tt(on_[:], o_[:], rb, ALU.mult, eng="pool")
        P.tt(fin_[:], W(on_[:], on_.h[:, :, :].rearrange("p h d -> p (h d)")), Gc[s_][:, mx * 1024:(mx + 1) * 1024], ALU.mult)

    def ph6(it):
        c, mx = it // 2, it % 2
        b = c // 4
        fin_ = fin[it % 3]
        stg_ = stg[0]
        for k in range(8):
            P.transpose(ptr[:, k, :], fin_[:, k * 128:(k + 1) * 128], C.identb[:])
        P.copy(stg_[:, mx * 8:(mx + 1) * 8, (c % 4) * 128:(c % 4 + 1) * 128], ptr[:], eng="act")
        if c % 4 == 3 and mx == 1:
            P.dma(V(mixT.h[:, b * 512:(b + 1) * 512].rearrange("(k p) t -> p k t", p=128), [(mixT.name, b)]), stg_[:])

    pipeline(2 * NT, [ph0, ph1, ph2, ph3, ph4, ph5, ph6])
    P.release(mC)
    if "yc" in C.tap_req:
        tap(C, "yc", mixT[:], [2048, L], BF16)
        P.flush(barrier=True)
    if C.stop_after == "l1mix":
        P.release(m0)
        return
    out_proj_run(C, wo1, 16)
    P.release(m0)
```

```python
import numpy as np
import ml_dtypes
from concourse.bass_utils import run_bass_kernel_spmd
import numpy as np
import concourse.bass as bass
import concourse.mybir as mybir

F32 = mybir.dt.float32
BF16 = mybir.dt.bfloat16
I32 = mybir.dt.int32
AF = mybir.ActivationFunctionType
ALU = mybir.AluOpType
AX = mybir.AxisListType

COMPUTE = ("pe", "act", "dve", "pool")
QUEUES = ("sp", "act", "pool")
ALLENG = ("pe", "act", "dve", "pool", "sp")


class V:
    __slots__ = ("ap", "keys")

    def __init__(self, ap, keys):
        self.ap = ap
        self.keys = tuple(keys)


class T:
    def __init__(self, name, handle, nsub=None):
        self.name = name
        self.h = handle
        self.nsub = nsub

    def __getitem__(self, idx):
        if self.nsub is None:
            return V(self.h[idx], [(self.name,)])
        return V(self.h[idx], [(self.name, i) for i in range(self.nsub)])

    def s(self, *subs):
        return _Sub(self, subs)


class _Sub:
    def __init__(self, t, subs):
        self.t = t
        self.subs = subs

    def __getitem__(self, idx):
        return V(self.t.h[idx], [(self.t.name, s) for s in self.subs])


def W(v, ap):
    return V(ap, v.keys)


class Op:
    __slots__ = ("eng", "fn", "reads", "writes", "is_dma", "idx", "waits",
                 "signal", "semval", "dsem", "dval", "vc")


class Prog:
    def __init__(self, nc, n_dma_sems=(16, 4, 16)):
        self.nc = nc
        self.ops = []
        self.eng_obj = {"pe": nc.tensor, "act": nc.scalar, "dve": nc.vector,
                        "pool": nc.gpsimd, "sp": nc.sync}
        self.n_dma_sems = dict(zip(QUEUES, n_dma_sems))
        self._stack = []
        self._names = set()
        self._keep = []
        self.cnt = {e: 0 for e in ALLENG}
        self.sc = {e: 0 for e in COMPUTE}
        self.last_writer = {}
        self.readers = {}
        self.dma_rr = {q: 0 for q in QUEUES}
        self.dma_tot = {}
        self.dma_last = {}
        self.evc = {e: {} for e in ALLENG}
        self.sems = {}
        for e in COMPUTE:
            g = nc.semaphore("s_" + e)
            self.sems[e] = g.__enter__()
            self._keep.append(g)
        self.dsems = {}
        for q in QUEUES:
            for s in range(self.n_dma_sems[q]):
                g = nc.semaphore(f"d_{q}_{s}")
                self.dsems[(q, s)] = g.__enter__()
                self._keep.append(g)
        self.n_wait = 0
        self.n_ins = 0

    def _uniq(self, name):
        n = name
        i = 0
        while n in self._names:
            i += 1
            n = f"{name}_{i}"
        self._names.add(n)
        return n

    def sbuf(self, name, shape, dtype, nsub=None):
        name = self._uniq(name)
        g = self.nc.sbuf_tensor(name, list(shape), dtype)
        h = g.__enter__()
        self._stack.append(g)
        return T(name, h, nsub)

    def psum(self, name, shape, dtype, nsub=None):
        name = self._uniq(name)
        g = self.nc.psum_tensor(name, list(shape), dtype)
        h = g.__enter__()
        self._stack.append(g)
        return T(name, h, nsub)

    def dram(self, name, shape, dtype, kind="Internal", nsub=None):
        name = self._uniq(name)
        h = self.nc.dram_tensor(name, list(shape), dtype, kind=kind)
        return T(name, h.ap() if hasattr(h, "ap") else h, nsub)

    def mark(self):
        return len(self._stack)

    def release(self, mark):
        self.flush(barrier=True)
        while len(self._stack) > mark:
            g = self._stack.pop()
            g.__exit__(None, None, None)

    def add(self, eng, fn, reads, writes, is_dma=False):
        op = Op()
        op.eng = eng
        op.fn = fn
        rk = []
        for r in reads:
            if r is None:
                continue
            rk.extend(r.keys)
        wk = []
        for w in writes:
            if w is None:
                continue
            wk.extend(w.keys)
        op.reads = rk
        op.writes = wk
        op.is_dma = is_dma
        self.ops.append(op)
        return op

    def dma(self, out, in_, q="sp", **kw):
        e = self.eng_obj[q]
        return self.add(q, lambda: e.dma_start(out=out.ap, in_=in_.ap, **kw),
                        [in_], [out], is_dma=True)

    def mm(self, out, lhsT, rhs, start=True, stop=True, **kw):
        nc = self.nc
        return self.add("pe", lambda: nc.tensor.matmul(out.ap, lhsT.ap, rhs.ap, start=start, stop=stop, **kw),
                        [lhsT, rhs], [out])

    def transpose(self, out, in_, ident):
        nc = self.nc
        return self.add("pe", lambda: nc.tensor.transpose(out.ap, in_.ap, ident.ap), [in_, ident], [out])

    def act(self, out, in_, func, bias=None, scale=None, accum_out=None):
        nc = self.nc
        kw = {}
        reads = [in_]
        if bias is not None:
            if isinstance(bias, V):
                kw["bias"] = bias.ap
                reads.append(bias)
            else:
                kw["bias"] = bias
        if scale is not None:
            if isinstance(scale, V):
                kw["scale"] = scale.ap
                reads.append(scale)
            else:
                kw["scale"] = scale
        writes = [out]
        if accum_out is not None:
            kw["accum_out"] = accum_out.ap
            writes.append(accum_out)
        return self.add("act", lambda: nc.scalar.activation(out.ap, in_.ap, func, **kw), reads, writes)

    def _veng(self, eng):
        return self.nc.vector if eng == "dve" else self.nc.gpsimd

    def copy(self, out, in_, eng="dve"):
        if eng == "act":
            nc = self.nc
            return self.add("act", lambda: nc.scalar.copy(out.ap, in_.ap), [in_], [out])
        e = self._veng(eng)
        return self.add(eng, lambda: e.tensor_copy(out.ap, in_.ap), [in_], [out])

    def memset(self, out, val, eng="dve"):
        e = self._veng(eng)
        return self.add(eng, lambda: e.memset(out.ap, val), [], [out])

    def tt(self, out, in0, in1, op, eng="dve"):
        e = self._veng(eng)
        return self.add(eng, lambda: e.tensor_tensor(out.ap, in0.ap, in1.ap, op), [in0, in1], [out])

    def ts(self, out, in0, s1, op0, s2=None, op1=None, eng="dve", accum_out=None):
        e = self._veng(eng)
        reads = [in0]
        a1 = s1
        a2 = s2
        if isinstance(s1, V):
            reads.append(s1)
            a1 = s1.ap
        if isinstance(s2, V):
            reads.append(s2)
            a2 = s2.ap
        kw = {}
        writes = [out]
        if op1 is not None:
            kw["op1"] = op1
        if accum_out is not None:
            kw["accum_out"] = accum_out.ap
            writes.append(accum_out)
        return self.add(eng, lambda: e.tensor_scalar(out.ap, in0.ap, a1, a2, op0, **kw), reads, writes)

    def stt(self, out, in0, scalar, in1, op0, op1, eng="dve", accum_out=None):
        e = self._veng(eng)
        reads = [in0, in1]
        a = scalar
        if isinstance(scalar, V):
            reads.append(scalar)
            a = scalar.ap
        kw = {}
        writes = [out]
        if accum_out is not None:
            kw["accum_out"] = accum_out.ap
            writes.append(accum_out)
        return self.add(eng, lambda: e.scalar_tensor_tensor(out.ap, in0.ap, a, in1.ap, op0, op1, **kw), reads, writes)

    def scan(self, out, d0, d1, initial, op0=ALU.mult, op1=ALU.add):
        nc = self.nc
        reads = [d0, d1]
        a = initial
        if isinstance(initial, V):
            reads.append(initial)
            a = initial.ap
        return self.add("dve", lambda: nc.vector.tensor_tensor_scan(out.ap, d0.ap, d1.ap, a, op0, op1), reads, [out])

    def recip(self, out, in_):
        nc = self.nc
        return self.add("dve", lambda: nc.vector.reciprocal(out.ap, in_.ap), [in_], [out])

    def _done_clock(self, op):
        if op.is_dma:
            return (("d",) + op.dsem, op.dval)
        return (op.eng, op.idx)

    def flush(self, barrier=False, final_wait_ops=()):
        nc = self.nc
        ops = self.ops
        self.ops = []
        last_writer = self.last_writer
        readers = self.readers
        for op in ops:
            self.cnt[op.eng] += 1
            op.idx = self.cnt[op.eng]
            op.waits = []
            op.signal = False
            vc = self.evc[op.eng]
            deps = []
            for k in op.reads:
                w = last_writer.get(k)
                if w is not None:
                    deps.append((w, "raw"))
            for k in op.writes:
                w = last_writer.get(k)
                if w is not None:
                    deps.append((w, "waw"))
                for r in readers.get(k, ()):
                    deps.append((r, "war"))
            if op.is_dma:
                q = op.eng
                slot = self.dma_rr[q] % self.n_dma_sems[q]
                self.dma_rr[q] += 1
                op.dsem = (q, slot)
                prev = self.dma_last.get((q, slot))
                if prev is not None:
                    deps.append((prev, "sem"))
                self.dma_tot[(q, slot)] = self.dma_tot.get((q, slot), 0) + 1
                op.dval = self.dma_tot[(q, slot)]
                self.dma_last[(q, slot)] = op
            seen = set()
            for d, kind in deps:
                if d is op or id(d) in seen:
                    continue
                seen.add(id(d))
                if (not d.is_dma) and (not op.is_dma) and d.eng == op.eng:
                    if kind == "waw" and d.eng == "pe":
                        continue
                cn, cv = self._done_clock(d)
                if vc.get(cn, 0) >= cv:
                    continue
                op.waits.append(d)
                vc[cn] = cv
                for n2, v2 in d.vc.items():
                    if vc.get(n2, 0) < v2:
                        vc[n2] = v2
                if not d.is_dma:
                    d.signal = True
            op.vc = dict(vc)
            for k in op.reads:
                readers.setdefault(k, []).append(op)
            for k in op.writes:
                last_writer[k] = op
                readers[k] = []
        fin = list(final_wait_ops)
        for d in fin:
            if not d.is_dma:
                d.signal = True
        for op in ops:
            if op.is_dma:
                continue
            if op.signal:
                self.sc[op.eng] += 1
            op.semval = self.sc[op.eng]
        for op in ops:
            e = self.eng_obj[op.eng]
            for d in op.waits:
                if d.is_dma:
                    e.wait_ge(self.dsems[d.dsem], 16 * d.dval)
                else:
                    e.wait_ge(self.sems[d.eng], d.semval)
                self.n_wait += 1
            ins = op.fn()
            self.n_ins += 1
            if op.is_dma:
                ins.then_inc(self.dsems[op.dsem], 16)
            elif op.signal:
                ins.then_inc(self.sems[op.eng], 1)
        for d in fin:
            if d.is_dma:
                nc.sync.wait_ge(self.dsems[d.dsem], 16 * d.dval)
            else:
                nc.sync.wait_ge(self.sems[d.eng], d.semval)
        if barrier:
            for e in COMPUTE:
                self.eng_obj[e].drain().then_inc(self.sems[e], 1)
                self.sc[e] += 1
            for e in ALLENG:
                eo = self.eng_obj[e]
                for e2 in COMPUTE:
                    if e2 != e:
                        eo.wait_ge(self.sems[e2], self.sc[e2])
                for (q, s), tot in self.dma_tot.items():
                    eo.wait_ge(self.dsems[(q, s)], 16 * tot)
            for e in ALLENG:
                vc = self.evc[e]
                for e2 in COMPUTE:
                    vc[e2] = self.cnt[e2]
                for (q, s), tot in self.dma_tot.items():
                    vc[("d", q, s)] = tot
            self.last_writer = {}
            self.readers = {}

    def close(self):
        while self._stack:
            g = self._stack.pop()
            g.__exit__(None, None, None)
        while self._keep:
            g = self._keep.pop()
            g.__exit__(None, None, None)


L = 2048
D = 1024
NT = L // 128
KT = D // 128
FF = 2816
FT = FF // 128
EPS = 1e-6
EVEN_IN = 3088
ODD_IN = 6160

WEIGHT_NAMES = ["norm_mix_g", "norm_ffn_g", "final_norm_g", "ev_in_w", "s5_lam_re", "s5_lam_im", "s5_log_step",
                "s5_b_re", "s5_b_im", "s5_c_re", "s5_c_im", "s5_d", "s5_glu_w", "s5_glu_b", "ssd_conv_w",
                "ssd_conv_b", "ssd_dt_bias", "ssd_a_log", "ssd_d", "ssd_norm_g", "ev_out_w", "od_in_w",
                "ret_norm_g", "gla_gate_w", "gla_gate_b", "gla_norm_g", "od_out_w", "ffn_gate_w", "ffn_up_w",
                "ffn_down_w"]


def host_consts():
    c = {}
    c["c_ident"] = np.eye(128, dtype=np.float32)
    k = np.arange(128)
    c["c_tri"] = (k[:, None] <= k[None, :]).astype(np.float32)
    c["c_up"] = (k[:, None] > k[None, :]).astype(np.float32)
    c["c_pos"] = np.broadcast_to(np.arange(L, dtype=np.float32)[None, :], (128, L)).copy()
    c["c_pidx"] = np.arange(128, dtype=np.float32)[:, None].copy()
    c["c_dmod"] = (np.arange(128) % 64).astype(np.float32)[:, None].copy()
    c["c_sign"] = np.where(np.arange(128) < 64, -1.0, 1.0).astype(np.float32)[:, None].copy()
    c["c_gmask"] = (np.arange(128)[:, None] // 16 == np.arange(8)[None, :]).astype(np.float32)
    return c


class Ctx:
    pass


def rearr(t, pattern, **kw):
    return V(t.h.rearrange(pattern, **kw), [(t.name,)])


def build(taps=(), stop_after=None, skip_mix=False, dbg=0, skip_l0=False):
    nc = bass.Bass("TRN2", target_bir_lowering=False)
    P = Prog(nc)
    C = Ctx()
    C.P = P
    C.nc = nc
    spec = {"x": [L, D], "norm_mix_g": [2, D], "norm_ffn_g": [2, D], "final_norm_g": [D],
            "ev_in_w": [1, D, EVEN_IN], "s5_lam_re": [1, 32, 64], "s5_lam_im": [1, 32, 64], "s5_log_step": [1, 32],
            "s5_b_re": [1, 32, 64, 16], "s5_b_im": [1, 32, 64, 16], "s5_c_re": [1, 32, 16, 64],
            "s5_c_im": [1, 32, 16, 64], "s5_d": [1, 32, 16], "s5_glu_w": [1, 512, 512], "s5_glu_b": [1, 512],
            "ssd_conv_w": [1, 4, 1536], "ssd_conv_b": [1, 1536], "ssd_dt_bias": [1, 16], "ssd_a_log": [1, 16],
            "ssd_d": [1, 16], "ssd_norm_g": [1, 1024], "ev_out_w": [1, 1536, D], "od_in_w": [1, D, ODD_IN],
            "ret_norm_g": [1, 1024], "gla_gate_w": [1, 16, 512], "gla_gate_b": [1, 512], "gla_norm_g": [1, 1024],
            "od_out_w": [1, 2048, D], "ffn_gate_w": [2, D, FF], "ffn_up_w": [2, D, FF], "ffn_down_w": [2, FF, D]}
    I = {}
    for n, s in spec.items():
        I[n] = P.dram(n, s, F32, kind="ExternalInput")
    for n, a in host_consts().items():
        I[n] = P.dram(n, list(a.shape), F32, kind="ExternalInput")
    C.I = I
    out = P.dram("out", [L, D], F32, kind="ExternalOutput")
    C.taps = {}
    C.tap_req = taps
    C.stop_after = stop_after
    C.dbg = dbg

    C.hT = P.sbuf("hT", [128, KT, L], F32, nsub=NT)
    C.identf = P.sbuf("identf", [128, 128], F32)
    C.identb = P.sbuf("identb", [128, 128], BF16)
    C.onesb = P.sbuf("onesb", [128, 128], BF16)
    C.gcols = P.sbuf("gcols", [128, 5, KT], F32)
    P.dma(C.identf[:], I["c_ident"][:])
    P.copy(C.identb[:], C.identf[:])
    P.memset(C.onesb[:], 1.0)
    for i in range(2):
        P.dma(C.gcols[:, i, :], V(I["norm_mix_g"].h.rearrange("l (k p) -> l p k", p=128)[i], [("norm_mix_g",)]), allow_slow_non_contiguous=True)
        P.dma(C.gcols[:, 2 + i, :], V(I["norm_ffn_g"].h.rearrange("l (k p) -> l p k", p=128)[i], [("norm_ffn_g",)]), allow_slow_non_contiguous=True)
    P.dma(C.gcols[:, 4, :], rearr(I["final_norm_g"], "(k p) -> p k", p=128), allow_slow_non_contiguous=True)

    load_x(C)
    if stop_after == "load":
        return finish(C, out)
    for layer in range(2):
        if not skip_mix:
            if layer == 0:
                if skip_l0:
                    continue
                layer0_mixer(C)
                if "h0" in taps:
                    t_ = P.dram("tap_h0", [128, KT, L], F32, kind="ExternalOutput")
                    P.dma(t_[:], C.hT[:])
                    P.flush(barrier=True)
                if stop_after in ("A", "s5", "s5setup", "ssd", "mix0"):
                    return finish(C, out)
            else:
                layer1_mixer(C)
                if "h1" in taps:
                    t_ = P.dram("tap_h1", [128, KT, L], F32, kind="ExternalOutput")
                    P.dma(t_[:], C.hT[:])
                    P.flush(barrier=True)
                if stop_after in ("mix1", "l1mix"):
                    return finish(C, out)
        ffn(C, layer)
    final(C, out)
    return finish(C, out, done=True)


def finish(C, out, done=False):
    P = C.P
    P.flush(barrier=True)
    return C


def hT_v(C, k, t0, n):
    subs = list(range(t0 // 128, (t0 + n + 127) // 128))
    if k is None:
        return C.hT.s(*subs)[:, :, t0:t0 + n]
    return C.hT.s(*subs)[:, k, t0:t0 + n]


def load_x(C):
    P, I = C.P, C.I
    m = P.mark()
    xt = [P.sbuf("xt", [128, D], F32) for _ in range(3)]
    ps = [P.psum("pst", [128, 4, 128], F32) for _ in range(4)]
    n = 0
    for t in range(NT):
        xb = xt[t % 3]
        P.dma(xb[:], I["x"][t * 128:(t + 1) * 128, :])
        for half in range(2):
            pp = ps[n % 4]
            n += 1
            for j in range(4):
                k = half * 4 + j
                P.transpose(pp[:, j, :], xb[:, k * 128:(k + 1) * 128], C.identf[:])
            dst = C.hT.s(t)[:, half * 4:(half + 1) * 4, t * 128:(t + 1) * 128]
            if half == 0:
                P.copy(dst, pp[:], eng="dve")
            else:
                P.copy(dst, pp[:], eng="act")
    P.release(m)


def norm_T(C, gi, hnT):
    P = C.P
    sq = [P.sbuf("sq", [128, KT, 512], BF16) for _ in range(2)]
    ssp = [P.psum("ssp", [128, 512], F32) for _ in range(2)]
    rs = [P.sbuf("rs", [128, 512], F32) for _ in range(2)]
    for b in range(L // 512):
        t0 = b * 512
        s = sq[b % 2]
        pp = ssp[b % 2]
        r = rs[b % 2]
        P.act(s[:], hT_v(C, None, t0, 512), AF.Square)
        for k in range(KT):
            P.mm(pp[:], C.onesb[:], s[:, k, :], start=(k == 0), stop=(k == KT - 1))
        P.act(r[:], pp[:], AF.Sqrt, bias=EPS, scale=1.0 / D)
        P.recip(r[:], r[:])
        for k in range(KT):
            P.stt(hnT.s(b)[:, k, t0:t0 + 512], hT_v(C, k, t0, 512), C.gcols[:, gi, k:k + 1], r[:],
                  ALU.mult, ALU.mult)


def load_w_tile(C, buf, wT, k_tiles, c0, ncols, q="pool"):
    src = V(wT.h.rearrange("(k p) f -> p k f", p=128)[:, :, c0:c0 + ncols], [(wT.name,)])
    C.P.dma(buf[:, 0:k_tiles, 0:ncols], src, q=q)


def dram2d(t, idx):
    return T(t.name, t.h[idx])


def ffn(C, layer):
    P, I = C.P, C.I
    m = P.mark()
    hnT = P.sbuf("hnT", [128, KT, L], BF16, nsub=L // 512)
    m2 = P.mark()
    norm_T(C, 2 + layer, hnT)
    P.release(m2)
    Wg = dram2d(I["ffn_gate_w"], layer)
    Wu = dram2d(I["ffn_up_w"], layer)
    Wd = dram2d(I["ffn_down_w"], layer)
    HALF = 1024
    aT = P.sbuf("aT", [128, FT, HALF], BF16, nsub=FT)
    wg = [P.sbuf("wg", [128, KT, 128], BF16) for _ in range(3)]
    wu = [P.sbuf("wu", [128, KT, 128], BF16) for _ in range(3)]
    wd = [P.sbuf("wd", [128, FT, 128], BF16) for _ in range(2)]
    gps = [P.psum("gps", [128, 512], F32) for _ in range(2)]
    ups = [P.psum("ups", [128, 512], F32) for _ in range(2)]
    dps = [P.psum("dps", [128, 512], F32) for _ in range(2)]
    sg = [P.sbuf("sg", [128, 512], F32) for _ in range(2)]
    n = 0
    nd = 0
    for half in range(L // HALF):
        for ft in range(FT):
            g = wg[ft % 3]
            u = wu[ft % 3]
            load_w_tile(C, g, Wg, KT, ft * 128, 128)
            load_w_tile(C, u, Wu, KT, ft * 128, 128)
            for blk in range(HALF // 512):
                t0 = half * HALF + blk * 512
                b = t0 // 512
                gp = gps[n % 2]
                up = ups[n % 2]
                s = sg[n % 2]
                n += 1
                for k in range(KT):
                    P.mm(gp[:], g[:, k, :], hnT.s(b)[:, k, t0:t0 + 512], start=(k == 0), stop=(k == KT - 1))
                for k in range(KT):
                    P.mm(up[:], u[:, k, :], hnT.s(b)[:, k, t0:t0 + 512], start=(k == 0), stop=(k == KT - 1))
                P.act(s[:], gp[:], AF.Silu)
                P.tt(aT.s(ft)[:, ft, blk * 512:(blk + 1) * 512], up[:], s[:], ALU.mult)
        for dt_ in range(KT):
            w = wd[dt_ % 2]
            load_w_tile(C, w, Wd, FT, dt_ * 128, 128)
            for blk in range(HALF // 512):
                t0 = half * HALF + blk * 512
                dp = dps[nd % 2]
                nd += 1
                for f in range(FT):
                    P.mm(dp[:], w[:, f, :], aT.s(f)[:, f, blk * 512:(blk + 1) * 512], start=(f == 0), stop=(f == FT - 1))
                hv = hT_v(C, dt_, t0, 512)
                P.tt(hv, dp[:], hv, ALU.add)
    P.release(m)


def final(C, out):
    P = C.P
    m = P.mark()
    sq = [P.sbuf("fsq", [128, KT, 512], BF16) for _ in range(2)]
    ssp = [P.psum("fssp", [128, 512], F32) for _ in range(2)]
    rs = [P.sbuf("frs", [128, 512], F32) for _ in range(2)]
    yn = [P.sbuf("fyn", [128, KT, 512], F32) for _ in range(2)]
    tp = [P.psum("ftp", [128, 4, 128], F32) for _ in range(4)]
    ot = [P.sbuf("fot", [128, D], F32) for _ in range(3)]
    n = 0
    outs = []
    for b in range(L // 512):
        t0 = b * 512
        s = sq[b % 2]
        pp = ssp[b % 2]
        r = rs[b % 2]
        y = yn[b % 2]
        P.act(s[:], hT_v(C, None, t0, 512), AF.Square)
        for k in range(KT):
            P.mm(pp[:], C.onesb[:], s[:, k, :], start=(k == 0), stop=(k == KT - 1))
        P.act(r[:], pp[:], AF.Sqrt, bias=EPS, scale=1.0 / D)
        P.recip(r[:], r[:])
        for k in range(KT):
            P.stt(y[:, k, :], hT_v(C, k, t0, 512), C.gcols[:, 4, k:k + 1], r[:], ALU.mult, ALU.mult)
        for tt_ in range(4):
            o = ot[(b * 4 + tt_) % 3]
            for half in range(2):
                pq = tp[n % 4]
                n += 1
                for j in range(4):
                    k = half * 4 + j
                    P.transpose(pq[:, j, :], y[:, k, tt_ * 128:(tt_ + 1) * 128], C.identf[:])
                dst = W(o[:], o.h[:, half * 512:(half + 1) * 512].rearrange("p (j c) -> p j c", j=4))
                if half == 0:
                    P.copy(dst, pq[:], eng="dve")
                else:
                    P.copy(dst, pq[:], eng="act")
            tok = t0 + tt_ * 128
            outs.append(P.dma(out[tok:tok + 128, :], o[:]))
    C.out_ops = outs
    P.release(m)


def make_in_maps(inputs, n_cores=8):
    consts = host_consts()
    maps = []
    for b in range(n_cores):
        mp = {"x": np.ascontiguousarray(inputs["x"][b])}
        for n in WEIGHT_NAMES:
            mp[n] = np.ascontiguousarray(inputs[n])
        mp.update(consts)
        maps.append(mp)
    return maps


_CACHE = {}


def kernel(**inputs):
    inputs = {k: np.asarray(v) for k, v in inputs.items()}
    if "nc" not in _CACHE:
        C = build()
        _CACHE["nc"] = C.nc
    nc = _CACHE["nc"]
    maps = make_in_maps(inputs, 8)
    res = run_bass_kernel_spmd(nc, maps, core_ids=list(range(8)))
    return np.stack([res.results[b]["out"] for b in range(8)], axis=0).astype(np.float32)

import math
TWO_PI = 2.0 * math.pi


def pipeline(n, phases):
    K = len(phases)
    for step in range(n + K - 1):
        for k in range(K - 1, -1, -1):
            i = step - k
            if 0 <= i < n:
                phases[k](i)


def load_w(C, dstV, wT, c0, ncols, q="pool"):
    src = V(wT.h.rearrange("(k p) f -> p k f", p=128)[:, :, c0:c0 + ncols], [(wT.name,)])
    C.P.dma(dstV, src, q=q)


def tap(C, name, srcV, shape, dtype):
    if name not in C.tap_req:
        return
    t = C.P.dram("tap_" + name, shape, dtype, kind="ExternalOutput")
    C.P.dma(t[:], srcV)


def range_reduce(C, x, tmp_f, tmp_i, out):
    P = C.P
    P.ts(tmp_f, x, 1.0 / TWO_PI, ALU.mult, None)
    P.copy(tmp_i, tmp_f)
    P.copy(tmp_f, tmp_i)
    P.stt(out, tmp_f, -TWO_PI, x, ALU.mult, ALU.add)
    P.ts(tmp_f, out, math.pi, ALU.is_gt, None)
    P.stt(out, tmp_f, -TWO_PI, out, ALU.mult, ALU.add)
    P.ts(tmp_f, out, -math.pi, ALU.is_lt, None)
    P.stt(out, tmp_f, TWO_PI, out, ALU.mult, ALU.add)


def layer0_mixer(C):
    P, I = C.P, C.I
    m0 = P.mark()
    uT = P.sbuf("uT", [128, 4, L], BF16, nsub=4)
    mixT = P.dram("d_mixT", [1536, L], BF16, nsub=4)
    dtda = P.sbuf("dtda", [128, NT, 32], F32, nsub=NT)
    d_zs = P.dram("d_zs", [L, 1024], BF16, nsub=NT)
    d_x = P.dram("d_x", [L, 1024], BF16, nsub=4)
    d_B = P.dram("d_B", [L, 256], BF16, nsub=4)
    d_BT = P.dram("d_BT", [256, L], BF16, nsub=4)
    d_CT = P.dram("d_CT", [256, L], BF16, nsub=4)
    C.uT, C.mixT, C.dtda = uT, mixT, dtda
    C.d_zs, C.d_x, C.d_B, C.d_BT, C.d_CT = d_zs, d_x, d_B, d_BT, d_CT
    Win = dram2d(I["ev_in_w"], 0)

    mA = P.mark()
    hnT = P.sbuf("hnT0", [128, KT, L], BF16, nsub=4)
    m2 = P.mark()
    norm_T(C, 0, hnT)
    P.release(m2)

    m2 = P.mark()
    wbuf = [P.sbuf("wA", [128, KT, 128], BF16) for _ in range(3)]
    pA = [P.psum("pA", [128, 512], F32) for _ in range(2)]
    n = 0
    wnat = [P.sbuf("wnat", [128, KT, 128], BF16) for _ in range(2)]
    for ft in range(4):
        w = wbuf[ft % 3]
        wn = wnat[ft % 2]
        load_w(C, wn[:], Win, ft * 128, 128)
        for h in range(2):
            dst = W(w[:], w.h[:, :, h * 64:(h + 1) * 64].rearrange("p k (gl m) -> p k gl m", gl=4, m=16))
            srcv = W(wn[:], wn.h[:, :, :].rearrange("p k (gl h m) -> p k gl h m", gl=4, h=2, m=16)[:, :, :, h, :])
            P.copy(dst, srcv, eng="pool")
        for b in range(4):
            pp = pA[n % 2]
            n += 1
            for k in range(KT):
                P.mm(pp[:], w[:, k, :], hnT.s(b)[:, k, b * 512:(b + 1) * 512], start=(k == 0), stop=(k == KT - 1))
            P.copy(uT.s(b)[:, ft, b * 512:(b + 1) * 512], pp[:], eng="act")
    P.release(m2)

    m2 = P.mark()
    wz = [P.sbuf("wz", [128, KT, 512], BF16) for _ in range(2)]
    load_w(C, wz[0][:], Win, 512, 512)
    load_w(C, wz[1][:], Win, 1024, 512)
    zst = [P.sbuf("zst", [128, 1024], BF16) for _ in range(2)]
    pz = [P.psum("pz", [128, 512], F32) for _ in range(4)]
    n = 0
    for t in range(NT):
        st = zst[t % 2]
        for c in range(2):
            pp = pz[n % 4]
            n += 1
            for k in range(KT):
                P.mm(pp[:], hnT.s(t // 4)[:, k, t * 128:(t + 1) * 128], wz[c][:, k, :], start=(k == 0), stop=(k == KT - 1))
            P.act(st[:, c * 512:(c + 1) * 512], pp[:], AF.Silu)
        P.dma(d_zs.s(t)[t * 128:(t + 1) * 128, :], st[:])
    P.release(m2)

    m2 = P.mark()
    wdt = P.sbuf("wdt", [128, KT, 16], BF16)
    load_w(C, wdt[:], Win, 3072, 16)
    dtb = P.sbuf("dtb", [128, 16], F32)
    negA = P.sbuf("negA", [128, 16], F32)
    P.dma(dtb[:], V(I["ssd_dt_bias"].h[0].partition_broadcast(128), [("ssd_dt_bias",)]))
    P.dma(negA[:], V(I["ssd_a_log"].h[0].partition_broadcast(128), [("ssd_a_log",)]))
    P.act(negA[:], negA[:], AF.Exp)
    P.ts(negA[:], negA[:], -1.0, ALU.mult, None)
    pdt = [P.psum("pdt", [128, 16], F32) for _ in range(2)]
    dtt = [P.sbuf("dtt", [128, 16], F32) for _ in range(2)]
    for t in range(NT):
        pp = pdt[t % 2]
        tmp = dtt[t % 2]
        for k in range(KT):
            P.mm(pp[:], hnT.s(t // 4)[:, k, t * 128:(t + 1) * 128], wdt[:, k, :], start=(k == 0), stop=(k == KT - 1))
        P.tt(tmp[:], pp[:], dtb[:], ALU.add)
        P.act(tmp[:], tmp[:], AF.Exp)
        P.act(dtda.s(t)[:, t, 0:16], tmp[:], AF.Ln, bias=1.0)
        P.tt(dtda.s(t)[:, t, 16:32], dtda.s(t)[:, t, 0:16], negA[:], ALU.mult)
    P.release(m2)

    m2 = P.mark()
    wx = P.sbuf("wx", [128, KT, 1536], BF16)
    for j in range(3):
        load_w(C, wx[:, :, j * 512:(j + 1) * 512], Win, 1536 + j * 512, 512)
    cw = P.sbuf("cw", [128, 12, 4], F32)
    cb = P.sbuf("cb", [128, 12], F32)
    for k in range(4):
        P.dma(cw[:, :, k], V(I["ssd_conv_w"].h[0, k].rearrange("(t p) -> p t", p=128), [("ssd_conv_w",)]),
              allow_slow_non_contiguous=True)
    P.dma(cb[:], V(I["ssd_conv_b"].h[0].rearrange("(t p) -> p t", p=128), [("ssd_conv_b",)]),
          allow_slow_non_contiguous=True)
    xr = P.sbuf("xr", [128, 12, 515], F32, nsub=12)
    P.memset(xr[:], 0.0)
    acc = [P.sbuf("acc", [128, 512], F32) for _ in range(2)]
    xc = [P.sbuf("xc", [128, 512], BF16) for _ in range(3)]
    stg = [P.sbuf("stg", [128, 4, 1280], BF16) for _ in range(2)]
    ptx = [P.psum("ptx", [128, 4, 128], BF16) for _ in range(2)]
    px = [P.psum("px", [128, 512], F32) for _ in range(2)]
    def q0(it):
        b, ft = it // 12, it % 12
        pp = px[it % 2]
        for k in range(KT):
            P.mm(pp[:], wx[:, k, ft * 128:(ft + 1) * 128], hnT.s(b)[:, k, b * 512:(b + 1) * 512],
                 start=(k == 0), stop=(k == KT - 1))

    def q1(it):
        b, ft = it // 12, it % 12
        pp = px[it % 2]
        a = acc[it % 2]
        if b > 0:
            P.copy(xr.s(ft)[:, ft, 0:3], xr.s(ft)[:, ft, 512:515])
        P.copy(xr.s(ft)[:, ft, 3:515], pp[:], eng="act")
        P.act(a[:], xr.s(ft)[:, ft, 0:512], AF.Identity, bias=cb[:, ft:ft + 1], scale=cw[:, ft, 0:1])

    def q2(it):
        b, ft = it // 12, it % 12
        a = acc[it % 2]
        x_ = xc[it % 3]
        for k in range(1, 4):
            P.stt(a[:], xr.s(ft)[:, ft, k:k + 512], cw[:, ft, k:k + 1], a[:], ALU.mult, ALU.add)
        P.act(x_[:], a[:], AF.Silu)

    def q3(it):
        b, ft = it // 12, it % 12
        x_ = xc[it % 3]
        pt = ptx[it % 2]
        sg_ = stg[b % 2]
        if ft < 10:
            for j in range(4):
                P.transpose(pt[:, j, :], x_[:, j * 128:(j + 1) * 128], C.identb[:])
            P.copy(sg_[:, :, ft * 128:(ft + 1) * 128], pt[:])
        if ft >= 8:
            dst = d_BT if ft < 10 else d_CT
            r0 = (ft % 2) * 128
            P.dma(dst.s(b)[r0:r0 + 128, b * 512:(b + 1) * 512], x_[:])
        if ft == 11:
            P.dma(V(d_x.h[b * 512:(b + 1) * 512, :].rearrange("(j p) c -> p j c", p=128), [(d_x.name, b)]),
                  sg_[:, :, 0:1024])
            P.dma(V(d_B.h[b * 512:(b + 1) * 512, :].rearrange("(j p) c -> p j c", p=128), [(d_B.name, b)]),
                  sg_[:, :, 1024:1280])

    pipeline(48, [q0, q1, q2, q3])
    P.release(m2)
    P.release(mA)
    if "A" in C.tap_req:
        tap(C, "A", uT[:], [128, 4, L], BF16)
        tap(C, "A", dtda[:], [128, NT, 32], F32) if False else None
        t2 = P.dram("tap_dtda", [128, NT, 32], F32, kind="ExternalOutput")
        P.dma(t2[:], dtda[:])
        for nm, src, shp in (("zs", d_zs, [L, 1024]), ("x", d_x, [L, 1024]), ("B", d_B, [L, 256]), ("BT", d_BT, [256, L]), ("CT", d_CT, [256, L])):
            t3 = P.dram("tap_" + nm, shp, BF16, kind="ExternalOutput")
            P.dma(t3[:], src[:])
        P.flush(barrier=True)
    if C.stop_after == "A":
        P.release(m0)
        return

    if C.dbg != 99:
        s5_stage(C)
    P.flush(barrier=True)
    if "ya" in C.tap_req:
        tap(C, "ya", mixT[0:512, :], [512, L], BF16)
        P.flush(barrier=True)
    if C.stop_after in ("s5", "s5setup"):
        P.release(m0)
        return
    wo0 = out_proj_load(C, dram2d(I["ev_out_w"], 0), 12, s5_perm=True)
    ssd_stage(C)
    P.flush(barrier=True)
    if "yb" in C.tap_req:
        tap(C, "yb", mixT[512:1536, :], [1024, L], BF16)
        P.flush(barrier=True)
    if C.stop_after == "ssd":
        P.release(m0)
        return
    out_proj_run(C, wo0, 12)
    P.release(m0)


def out_proj_load(C, Wout, nk, s5_perm=False):
    P = C.P
    wo = P.sbuf("wo", [128, nk, D], BF16)
    src = Wout.h.rearrange("(k p) f -> p k f", p=128)
    if s5_perm:
        for k in range(4, nk):
            P.dma(wo[:, k, :], V(src[:, k, :], [(Wout.name,)]), q="pool")
        for k in range(4):
            for h in range(2):
                for gl in range(4):
                    r0 = k * 128 + gl * 32 + h * 16
                    P.dma(wo[h * 64 + gl * 16:h * 64 + gl * 16 + 16, k, :],
                          V(Wout.h[r0:r0 + 16, :], [(Wout.name,)]), q="pool")
    else:
        for k in range(nk):
            P.dma(wo[:, k, :], V(src[:, k, :], [(Wout.name,)]), q="pool")
    return wo


def out_proj_run(C, wo, nk):
    P = C.P
    m = P.mark()
    mixT = C.mixT
    mb = [P.sbuf("mb", [128, nk, 512], BF16) for _ in range(2)]
    po = [P.psum("po", [128, 512], F32) for _ in range(2)]
    n = 0
    for b in range(4):
        mb_ = mb[b % 2]
        P.dma(mb_[:], V(mixT.h.rearrange("(k p) t -> p k t", p=128)[:, :, b * 512:(b + 1) * 512], [(mixT.name, b)]))
        for dt_ in range(KT):
            pp = po[n % 2]
            n += 1
            for k in range(nk):
                P.mm(pp[:], wo[:, k, dt_ * 128:(dt_ + 1) * 128], mb_[:, k, :], start=(k == 0), stop=(k == nk - 1))
            hv = hT_v(C, dt_, b * 512, 512)
            P.tt(hv, pp[:], hv, ALU.add)
    P.release(m)


def s5_stage(C):
    P, I = C.P, C.I
    uT, mixT = C.uT, C.mixT
    m = P.mark()
    def pt(name, n=16):
        return P.sbuf(name, [128, n], F32)
    lr, li, stp = pt("lr"), pt("li"), pt("stp")
    for h in range(2):
        P.dma(lr[h * 64:(h + 1) * 64, :], V(I["s5_lam_re"].h[0].rearrange("(G h) p -> h p G", h=2)[h], [("s5_lam_re",)]),
              allow_slow_non_contiguous=True)
        P.dma(li[h * 64:(h + 1) * 64, :], V(I["s5_lam_im"].h[0].rearrange("(G h) p -> h p G", h=2)[h], [("s5_lam_im",)]),
              allow_slow_non_contiguous=True)
        P.dma(stp[h * 64:(h + 1) * 64, :],
              V(I["s5_log_step"].h[0].rearrange("(G h) -> h G", h=2)[h].partition_broadcast(64), [("s5_log_step",)]),
              allow_slow_non_contiguous=True)
    P.act(stp[:], stp[:], AF.Exp)
    lrs, th, mag = pt("lrs"), pt("th"), pt("mag")
    P.tt(lrs[:], lr[:], stp[:], ALU.mult)
    P.tt(th[:], li[:], stp[:], ALU.mult)
    P.act(mag[:], lrs[:], AF.Exp)
    tf, rr_, thc = pt("tf"), pt("rr"), pt("thc")
    ti = P.sbuf("ti", [128, 16], I32)
    cs, sn = pt("cs"), pt("sn")
    range_reduce(C, th[:], tf[:], ti[:], rr_[:])
    P.act(sn[:], rr_[:], AF.Sin)
    P.ts(thc[:], th[:], math.pi / 2, ALU.add, None)
    range_reduce(C, thc[:], tf[:], ti[:], rr_[:])
    P.act(cs[:], rr_[:], AF.Sin)
    are, aim = pt("are"), pt("aim")
    P.tt(are[:], mag[:], cs[:], ALU.mult)
    P.tt(aim[:], mag[:], sn[:], ALU.mult)
    den, am1, t1, t2, kre, kim = pt("den"), pt("am1"), pt("t1"), pt("t2"), pt("kre"), pt("kim")
    P.tt(den[:], lr[:], lr[:], ALU.mult)
    P.tt(t1[:], li[:], li[:], ALU.mult)
    P.tt(den[:], den[:], t1[:], ALU.add)
    P.recip(den[:], den[:])
    P.ts(am1[:], are[:], -1.0, ALU.add, None)
    P.tt(t1[:], am1[:], lr[:], ALU.mult)
    P.tt(t2[:], aim[:], li[:], ALU.mult)
    P.tt(t1[:], t1[:], t2[:], ALU.add)
    P.tt(kre[:], t1[:], den[:], ALU.mult)
    P.tt(t1[:], aim[:], lr[:], ALU.mult)
    P.tt(t2[:], am1[:], li[:], ALU.mult)
    P.tt(t1[:], t1[:], t2[:], ALU.subtract)
    P.tt(kim[:], t1[:], den[:], ALU.mult)
    cT, sT, nsT, thT = pt("cT"), pt("sT"), pt("nsT"), pt("thT")
    P.ts(thT[:], th[:], 128.0, ALU.mult, None)
    range_reduce(C, thT[:], tf[:], ti[:], rr_[:])
    P.act(sT[:], rr_[:], AF.Sin)
    P.ts(thc[:], thT[:], math.pi / 2, ALU.add, None)
    range_reduce(C, thc[:], tf[:], ti[:], rr_[:])
    P.act(cT[:], rr_[:], AF.Sin)
    P.ts(nsT[:], sT[:], -1.0, ALU.mult, None)
    cosT = P.sbuf("cosT", [128, 16, 512], F32)
    sinT = P.sbuf("sinT", [128, 16, 512], F32)
    c512, s512, ns512 = pt("c512"), pt("s512"), pt("ns512")
    bbre = P.sbuf("bbre", [128, 16, 16], BF16)
    bbim = P.sbuf("bbim", [128, 16, 16], BF16)
    Bpad = P.sbuf("Bpad", [128, 4, 8, 2, 64], BF16)
    Cpad = P.sbuf("Cpad", [128, 16, 2, 128], BF16)
    dcol = P.sbuf("dcol", [128, 4], F32)
    Dg = P.sbuf("Dg", [128, 4, 128], BF16)
    gw = P.sbuf("gw", [128, 4, 512], BF16)
    gbc = P.sbuf("gbc", [128, 4], F32)
    mtmp = P.mark()
    mtab = P.mark()
    pos = P.sbuf("pos", [128, 128], F32)
    P.dma(pos[:], I["c_pos"][:, 0:128])
    ang = P.sbuf("ang", [128, 16, 128], F32)
    tfb = P.sbuf("tfb", [128, 16, 128], F32)
    tib = P.sbuf("tib", [128, 16, 128], I32)
    rrb = P.sbuf("rrb", [128, 16, 128], F32)
    P.tt(ang[:], W(th[:], th.h[:, :].unsqueeze(2).to_broadcast([128, 16, 128])),
         W(pos[:], pos.h[:, :].unsqueeze(1).to_broadcast([128, 16, 128])), ALU.mult)
    range_reduce(C, ang[:], tfb[:], tib[:], rrb[:])
    P.act(sinT[:, :, 0:128], rrb[:], AF.Sin)
    P.ts(ang[:], ang[:], math.pi / 2, ALU.add, None)
    range_reduce(C, ang[:], tfb[:], tib[:], rrb[:])
    P.act(cosT[:, :, 0:128], rrb[:], AF.Sin)
    c256, s256, tq = pt("c256"), pt("s256"), pt("tq")
    P.tt(c256[:], cT[:], cT[:], ALU.mult)
    P.tt(tq[:], sT[:], sT[:], ALU.mult)
    P.tt(c256[:], c256[:], tq[:], ALU.subtract)
    P.tt(s256[:], cT[:], sT[:], ALU.mult)
    P.ts(s256[:], s256[:], 2.0, ALU.mult, None)
    P.tt(c512[:], c256[:], c256[:], ALU.mult)
    P.tt(tq[:], s256[:], s256[:], ALU.mult)
    P.tt(c512[:], c512[:], tq[:], ALU.subtract)
    P.tt(s512[:], c256[:], s256[:], ALU.mult)
    P.ts(s512[:], s512[:], 2.0, ALU.mult, None)
    P.ts(ns512[:], s512[:], -1.0, ALU.mult, None)
    e1, e2 = ang, tfb
    for (src0, dst0, cc, ss) in ((0, 128, cT, sT), (0, 256, c256, s256), (128, 384, c256, s256)):
        cb_ = W(cc[:], cc.h[:, :].unsqueeze(2).to_broadcast([128, 16, 128]))
        sb_ = W(ss[:], ss.h[:, :].unsqueeze(2).to_broadcast([128, 16, 128]))
        cs_ = cosT[:, :, src0:src0 + 128]
        sn_ = sinT[:, :, src0:src0 + 128]
        P.tt(e1[:], cs_, cb_, ALU.mult)
        P.tt(e2[:], sn_, sb_, ALU.mult, eng="pool")
        P.tt(cosT[:, :, dst0:dst0 + 128], e1[:], e2[:], ALU.subtract)
        P.tt(e1[:], sn_, cb_, ALU.mult)
        P.tt(e2[:], cs_, sb_, ALU.mult, eng="pool")
        P.tt(sinT[:, :, dst0:dst0 + 128], e1[:], e2[:], ALU.add)
    P.release(mtab)
    bre = P.sbuf("bre", [128, 16, 16], F32)
    bim = P.sbuf("bim", [128, 16, 16], F32)
    for h in range(2):
        P.dma(bre[h * 64:(h + 1) * 64, :, :],
              V(I["s5_b_re"].h[0].rearrange("(G h) p m -> h p G m", h=2)[h], [("s5_b_re",)]))
        P.dma(bim[h * 64:(h + 1) * 64, :, :],
              V(I["s5_b_im"].h[0].rearrange("(G h) p m -> h p G m", h=2)[h], [("s5_b_im",)]))
    kre_b = W(kre[:], kre.h[:, :].unsqueeze(2).to_broadcast([128, 16, 16]))
    kim_b = W(kim[:], kim.h[:, :].unsqueeze(2).to_broadcast([128, 16, 16]))
    u1 = P.sbuf("u1", [128, 16, 16], F32)
    u2 = P.sbuf("u2", [128, 16, 16], F32)
    P.tt(u1[:], bre[:], kre_b, ALU.mult)
    P.tt(u2[:], bim[:], kim_b, ALU.mult)
    P.tt(bbre[:], u1[:], u2[:], ALU.subtract)
    P.tt(u1[:], bim[:], kre_b, ALU.mult)
    P.tt(u2[:], bre[:], kim_b, ALU.mult)
    P.tt(bbim[:], u1[:], u2[:], ALU.add)
    gmask = P.sbuf("gmask", [128, 8], F32)
    P.dma(gmask[:], I["c_gmask"][:])
    ptb = [P.psum("ptb", [128, 64], BF16) for _ in range(2)]
    n = 0
    for T_ in range(4):
        for ri, bb in enumerate((bbre, bbim)):
            pp = ptb[n % 2]
            n += 1
            for h in range(2):
                src = W(bb[:], bb.h[h * 64:(h + 1) * 64, T_ * 4:(T_ + 1) * 4, :].rearrange("p g m -> p (g m)"))
                P.transpose(pp[h * 64:(h + 1) * 64, :], src, C.identb[h * 64:(h + 1) * 64, h * 64:(h + 1) * 64])
            for g8 in range(8):
                P.ts(Bpad[:, T_, g8, ri, :], pp[:], gmask[:, g8:g8 + 1], ALU.mult, None)
    cN = P.sbuf("cN", [128, 4, 2, 64], F32)
    for ri, nm in enumerate(("s5_c_re", "s5_c_im")):
        for T_ in range(4):
            for h in range(2):
                for gl in range(4):
                    g = (T_ * 4 + gl) * 2 + h
                    P.dma(cN[h * 64 + gl * 16:h * 64 + gl * 16 + 16, T_, ri, :], V(I[nm].h[0, g], [(nm,)]))
    cNb = P.sbuf("cNb", [128, 4, 2, 64], BF16)
    P.copy(cNb[:], cN[:])
    P.memset(Cpad[:], 0.0)
    ptc = [P.psum("ptc", [128, 128], BF16) for _ in range(2)]
    n = 0
    for T_ in range(4):
        for ri in range(2):
            pp = ptc[n % 2]
            n += 1
            for h in range(2):
                P.transpose(pp[h * 64:(h + 1) * 64, :], cNb[:, T_, ri, :], C.identb[:])
            for h in range(2):
                for gl in range(4):
                    G = T_ * 4 + gl
                    c0 = h * 64 + gl * 16
                    if ri == 0:
                        P.copy(Cpad[h * 64:(h + 1) * 64, G, 0, c0:c0 + 16], pp[h * 64:(h + 1) * 64, c0:c0 + 16])
                    else:
                        P.ts(Cpad[h * 64:(h + 1) * 64, G, 1, c0:c0 + 16], pp[h * 64:(h + 1) * 64, c0:c0 + 16],
                             -1.0, ALU.mult, None)
    for h in range(2):
        for gl in range(4):
            src = I["s5_d"].h[0].rearrange("(T gl h) m -> gl h m T", gl=4, h=2)[gl, h]
            P.dma(dcol[h * 64 + gl * 16:h * 64 + gl * 16 + 16, :], V(src, [("s5_d",)]), allow_slow_non_contiguous=True)
    for T_ in range(4):
        P.ts(Dg[:, T_, :], C.identf[:], dcol[:, T_:T_ + 1], ALU.mult, None)
    Wg_ = dram2d(I["s5_glu_w"], 0)
    gwf = P.sbuf("gwf", [128, 4, 512], F32)
    for Ti in range(4):
        for h in range(2):
            for gl in range(4):
                r0 = Ti * 128 + gl * 32 + h * 16
                P.dma(gwf[h * 64 + gl * 16:h * 64 + gl * 16 + 16, Ti, :], V(Wg_.h[r0:r0 + 16, :], [(Wg_.name,)]))
    for Ti in range(4):
        for T_ in range(4):
            for h2 in range(2):
                dst = W(gw[:], gw.h[:, Ti, T_ * 128 + h2 * 64:T_ * 128 + (h2 + 1) * 64].rearrange("p (gl m) -> p gl m", gl=4))
                srcv = W(gwf[:], gwf.h[:, Ti, T_ * 128:(T_ + 1) * 128].rearrange("p (gl h m) -> p gl h m", gl=4, h=2)[:, :, h2, :])
                P.copy(dst, srcv, eng=("dve" if (T_ + h2) % 2 == 0 else "pool"))
    for h in range(2):
        for gl in range(4):
            src = I["s5_glu_b"].h[0].rearrange("(T gl h m) -> gl h m T", T=4, gl=4, h=2)[gl, h]
            P.dma(gbc[h * 64 + gl * 16:h * 64 + gl * 16 + 16, :], V(src, [("s5_glu_b",)]), allow_slow_non_contiguous=True)
    if "s5setup" in C.tap_req:
        for nm, src, shp, dt_ in (("cosT", cosT, [128, 16, 128], F32), ("sinT", sinT, [128, 16, 128], F32),
                                  ("are", are, [128, 16], F32), ("aim", aim, [128, 16], F32), ("kre", kre, [128, 16], F32),
                                  ("kim", kim, [128, 16], F32), ("cT", cT, [128, 16], F32), ("sT", sT, [128, 16], F32),
                                  ("Bpad", Bpad, [128, 4, 8, 2, 64], BF16), ("Cpad", Cpad, [128, 16, 2, 128], BF16),
                                  ("Dg", Dg, [128, 4, 128], BF16), ("gw", gw, [128, 4, 512], BF16), ("gbc", gbc, [128, 4], F32)):
            t3 = P.dram("tap_" + nm, shp, dt_, kind="ExternalOutput")
            P.dma(t3[:], src[:])
        P.flush(barrier=True)
    P.release(mtmp)
    print("sbuf remaining before s5 main", C.nc.sbuf_bytes_remaining)
    if C.stop_after == "s5setup":
        P.release(m)
        return
    psZ = [P.psum("psZ", [128, 2, 512], F32) for _ in range(2)]
    psY = [P.psum("psY", [128, 512], F32) for _ in range(2)]
    psG = [P.psum("psG", [128, 512], F32) for _ in range(2)]
    wre = [P.sbuf("wre", [128, 512], F32) for _ in range(2)]
    wim = [P.sbuf("wim", [128, 512], F32) for _ in range(2)]
    tb = [P.sbuf("tb", [128, 512], F32) for _ in range(2)]
    xpr = [P.sbuf("xpr", [128, 512], F32) for _ in range(2)]
    xpi = [P.sbuf("xpi", [128, 512], F32) for _ in range(2)]
    xre = [P.sbuf("xre", [128, 512], BF16) for _ in range(2)]
    xim = [P.sbuf("xim", [128, 512], BF16) for _ in range(2)]
    init = P.sbuf("init", [128, 16, 2], F32, nsub=16)
    tc_ = P.sbuf("tc", [128, 16, 2], F32, nsub=16)
    P.memset(init[:], 0.0)
    zb = [P.sbuf("zb", [128, 4, 512], BF16) for _ in range(1)]
    yst = [P.sbuf("yst", [128, 4, 512], BF16) for _ in range(1)]
    t4s = [P.sbuf("t4s", [128, 512], F32) for _ in range(2)]

    def v4(v, ap):
        return W(v, ap.rearrange("p (c t) -> p c t", c=4))

    def s5_iter(b, T_, gl, i2, py):
        G = T_ * 4 + gl
        pz_ = psZ[i2]
        for h in range(2):
            g8 = h * 4 + gl
            for ri in range(2):
                P.mm(pz_[h * 64:(h + 1) * 64, ri, :], Bpad[:, T_, g8, ri, :], uT.s(b)[:, T_, b * 512:(b + 1) * 512])
        yield
        cos_b = cosT[:, G, :]
        sin_b = sinT[:, G, :]
        zre4 = pz_[:, 0, :]
        zim4 = pz_[:, 1, :]
        wr_, wi_ = wre[i2], wim[i2]
        t1, t2, t3, t4 = wr_, tb[i2], wi_, t4s[i2]
        P.tt(t1[:], zre4, cos_b, ALU.mult)
        yield
        P.tt(t2[:], zim4, sin_b, ALU.mult)
        yield
        P.tt(t3[:], zim4, cos_b, ALU.mult)
        yield
        P.tt(t4[:], zre4, sin_b, ALU.mult)
        yield
        P.tt(wr_[:], t1[:], t2[:], ALU.add, eng="pool")
        yield
        P.tt(wi_[:], t3[:], t4[:], ALU.subtract, eng="pool")
        yield
        xr_, xi_ = xpr[i2], xpi[i2]
        rho_b = W(mag[:], mag.h[:, G:G + 1].to_broadcast([128, 512]))
        P.scan(xr_[:], rho_b, wr_[:], init.s(G)[:, G, 0:1])
        yield
        P.scan(xi_[:], rho_b, wi_[:], init.s(G)[:, G, 1:2])
        yield
        er = xr_[:, 511:512]
        ei = xi_[:, 511:512]
        P.act(tc_.s(G)[:, G, 0:1], er, AF.Copy, scale=c512[:, G:G + 1])
        yield
        P.act(tc_.s(G)[:, G, 1:2], er, AF.Copy, scale=s512[:, G:G + 1])
        yield
        P.act(init.s(G)[:, G, 0:1], ei, AF.Identity, scale=ns512[:, G:G + 1], bias=tc_.s(G)[:, G, 0:1])
        yield
        P.act(init.s(G)[:, G, 1:2], ei, AF.Identity, scale=c512[:, G:G + 1], bias=tc_.s(G)[:, G, 1:2])
        yield
        P.tt(t1[:], xr_[:], cos_b, ALU.mult)
        yield
        P.tt(t2[:], xi_[:], sin_b, ALU.mult)
        yield
        P.tt(t3[:], xr_[:], sin_b, ALU.mult, eng="pool")
        yield
        P.tt(t4[:], xi_[:], cos_b, ALU.mult, eng="pool")
        yield
        P.tt(xre[i2][:], t1[:], t2[:], ALU.subtract)
        yield
        P.tt(xim[i2][:], t3[:], t4[:], ALU.add, eng="pool")
        yield
        P.mm(py[:], Cpad[:, G, 0, :], xre[i2][:], start=(gl == 0), stop=False)
        P.mm(py[:], Cpad[:, G, 1, :], xim[i2][:], start=False, stop=False)
        yield

    def interleave(gens):
        gens = list(gens)
        while gens:
            nxt = []
            for g in gens:
                try:
                    next(g)
                    nxt.append(g)
                except StopIteration:
                    pass
            gens = nxt

    ny = 0
    for b in range(4):
        zb_ = zb[0]
        yst_ = yst[0]
        for T_ in range(4):
            py = psY[ny % 2]
            ny += 1
            for gp in range(2):
                interleave([s5_iter(b, T_, gp * 2 + 0, 0, py), s5_iter(b, T_, gp * 2 + 1, 1, py)])
            P.mm(py[:], Dg[:, T_, :], uT.s(b)[:, T_, b * 512:(b + 1) * 512], start=False, stop=True)
            P.act(zb_[:, T_, :], py[:], AF.Gelu)
        for To in range(4):
            pg = psG[To % 2]
            s_ = tb[To % 2]
            for Ti in range(4):
                P.mm(pg[:], gw[:, Ti, To * 128:(To + 1) * 128], zb_[:, Ti, :], start=(Ti == 0), stop=(Ti == 3))
            P.act(s_[:], pg[:], AF.Sigmoid, bias=gbc[:, To:To + 1])
            P.tt(yst_[:, To, :], zb_[:, To, :], s_[:], ALU.mult)
        P.dma(V(mixT.h[0:512, b * 512:(b + 1) * 512].rearrange("(k p) t -> p k t", p=128), [(mixT.name, b)]), yst_[:])
    P.release(m)


def ssd_stage(C):
    P, I = C.P, C.I
    mixT, dtda = C.mixT, C.dtda
    m = P.mark()
    tri = P.sbuf("tri", [128, 128], F32)
    up = P.sbuf("up", [128, 128], F32)
    onesf = P.sbuf("onesf", [128, 128], F32)
    Dt = P.sbuf("Dt", [128, 16], F32)
    ng = P.sbuf("ng", [128, 1024], F32)
    P.dma(tri[:], I["c_tri"][:])
    P.dma(up[:], I["c_up"][:])
    P.memset(onesf[:], 1.0)
    P.dma(Dt[:], V(I["ssd_d"].h[0].partition_broadcast(128), [("ssd_d",)]))
    P.dma(ng[:], V(I["ssd_norm_g"].h[0].partition_broadcast(128), [("ssd_norm_g",)]))
    state = P.sbuf("state", [64, 16, 64], F32)
    state_bf = P.sbuf("state_bf", [64, 16, 64], BF16)
    P.memset(state[:], 0.0)
    P.memset(state_bf[:], 0.0)
    xa_ = [P.sbuf("sxa", [128, 1024], BF16) for _ in range(2)]
    xb_ = [P.sbuf("sxb", [128, 1024], BF16) for _ in range(2)]
    Bc_ = [P.sbuf("sB", [128, 256], BF16) for _ in range(2)]
    zc_ = [P.sbuf("sz", [128, 1024], BF16) for _ in range(2)]
    BTc_ = [P.sbuf("sBT", [64, 4, 128], BF16) for _ in range(4)]
    CTc_ = [P.sbuf("sCT", [64, 4, 128], BF16) for _ in range(4)]
    R_ = [P.sbuf("R", [128, 16, 128], F32) for _ in range(1)]
    seg_ = [P.sbuf("seg", [128, 16, 128], F32) for _ in range(1)]
    sc_ = [P.sbuf("scT", [128, 16, 128], BF16) for _ in range(1)]
    CBm_ = [P.sbuf("CBm", [128, 4, 128], F32) for _ in range(1)]
    ec_ = [P.sbuf("ec", [128, 2, 16], F32) for _ in range(3)]
    xdt_ = [P.sbuf("xdt", [128, 1024], BF16) for _ in range(3)]
    xdd_ = [P.sbuf("xdd", [128, 1024], BF16) for _ in range(2)]
    Y = P.sbuf("Y", [128, 1024], F32)
    Tt = P.sbuf("Tt", [128, 1024], F32)
    junk = P.sbuf("junk", [128, 1024], BF16)
    Yn_ = [P.sbuf("Yn", [128, 1024], BF16) for _ in range(2)]
    ss = P.sbuf("ss", [128, 2], F32)
    ybT = [P.sbuf("ybT", [128, 8, 512], BF16) for _ in range(1)]
    pd = P.psum("pd", [128, 2, 512], F32)
    pY = P.psum("pY", [128, 4, 512], F32)
    pcb = P.psum("pcb", [128, 4, 128], F32)
    pmix = P.psum("pmix", [128, 512], F32)
    ptr = W(pmix[:], pmix.h[:, 0:256].bitcast(BF16).rearrange("p (k t) -> p k t", k=4))
    psm0 = W(pmix[:], pmix.h[:, 256:272])
    psm1 = W(pmix[:], pmix.h[:, 272:288])
    psm = W(pmix[:], pmix.h[:, 256:288].rearrange("p (a b) -> p a b", a=2))

    def hd(v, ap):
        return W(v, ap.rearrange("p (h d) -> p h d", h=16))

    def s0(c):
        b = c // 4
        tok = slice(c * 128, (c + 1) * 128)
        P.dma(xa_[c % 2][:], C.d_x.s(b)[tok, :])
        P.dma(BTc_[c % 4][:], V(C.d_BT.h[:, tok].rearrange("(g n) t -> n g t", n=64), [(C.d_BT.name, b)]))
        P.dma(CTc_[c % 4][:], V(C.d_CT.h[:, tok].rearrange("(g n) t -> n g t", n=64), [(C.d_CT.name, b)]))

    def s1(c):
        R = R_[0]
        da_v = dtda.s(c)[:, c, 16:32]
        dt_v = dtda.s(c)[:, c, 0:16]
        P.tt(R[:], W(tri[:], tri.h[:, :].unsqueeze(1).to_broadcast([128, 16, 128])),
             W(da_v, dtda.h[:, c, 16:32].unsqueeze(2).to_broadcast([128, 16, 128])), ALU.mult)
        x_c = xa_[c % 2]
        xdt = xdt_[c % 3]
        P.tt(hd(xdt[:], xdt.h[:, :]), hd(x_c[:], x_c.h[:, :]),
             W(dt_v, dtda.h[:, c, 0:16].unsqueeze(2).to_broadcast([128, 16, 64])), ALU.mult, eng="pool")

    def s2(c):
        R = R_[0]
        seg = seg_[0]
        ec = ec_[c % 3]
        CBm = CBm_[0]
        da_v = dtda.s(c)[:, c, 16:32]
        BT_c, CT_c = BTc_[c % 4], CTc_[c % 4]
        P.mm(psm0, tri[:], da_v)
        P.mm(psm1, onesf[:], da_v)
        for g in range(4):
            P.mm(pcb[:, g, :], BT_c[:, g, :], CT_c[:, g, :])
        for half in range(2):
            for q in range(2):
                hq = half * 2 + q
                P.mm(pd[:, q, :], up[:], W(R[:], R.h[:, hq * 4:(hq + 1) * 4, :].rearrange("p h i -> p (h i)")))
            if half == 0:
                P.act(ec[:], psm, AF.Exp)
            P.act(W(seg[:], seg.h[:, half * 8:(half + 1) * 8, :].rearrange("p h i -> p (h i)")),
                  W(pd[:], pd.h[:, :, :].rearrange("p q i -> p (q i)")), AF.Exp)
        P.tt(CBm[:], pcb[:], W(tri[:], tri.h[:, :].unsqueeze(1).to_broadcast([128, 4, 128])), ALU.mult)

    def s3(c):
        seg = seg_[0]
        sc = sc_[0]
        CBm = CBm_[0]
        xdt = xdt_[c % 3]
        xdd = xdd_[c % 2]
        b_ = c // 4
        tok = slice(c * 128, (c + 1) * 128)
        P.dma(xb_[c % 2][:], C.d_x.s(b_)[tok, :])
        P.dma(Bc_[c % 2][:], C.d_B.s(b_)[tok, :])
        P.dma(zc_[c % 2][:], C.d_zs.s(c)[tok, :])
        P.tt(W(sc[:], sc.h[:, :, :].rearrange("p (g h) i -> p g h i", g=4)),
             W(seg[:], seg.h[:, :, :].rearrange("p (g h) i -> p g h i", g=4)),
             W(CBm[:], CBm.h[:, :, :].unsqueeze(2).to_broadcast([128, 4, 4, 128])), ALU.mult)
        P.tt(hd(xdd[:], xdd.h[:, :]), hd(xdt[:], xdt.h[:, :]),
             W(seg[:], seg.h[:, :, 127:128].to_broadcast([128, 16, 64])), ALU.mult, eng="pool")

    def s4(c):
        sc = sc_[0]
        xdt = xdt_[c % 3]
        CT_c = CTc_[c % 4]
        for h in range(16):
            P.mm(pY[:, h // 8, (h % 8) * 64:(h % 8 + 1) * 64], sc[:, h, :], xdt[:, h * 64:(h + 1) * 64])
        for g in range(4):
            P.mm(pY[:, 2 + g // 2, (g % 2) * 256:(g % 2 + 1) * 256], CT_c[:, g, :],
                 W(state_bf[:], state_bf.h[:, g * 4:(g + 1) * 4, :].rearrange("p h d -> p (h d)")))

    def s5(c):
        ec = ec_[c % 3]
        x_c, z_c, B_c = xb_[c % 2], zc_[c % 2], Bc_[c % 2]
        xdd = xdd_[c % 2]
        Yn = Yn_[c % 2]
        yi = W(pY[:], pY.h[:, 2:4, :].rearrange("p q (h d) -> p (q h) d", d=64))
        ya_ = W(pY[:], pY.h[:, 0:2, :].rearrange("p q i -> p (q i)"))
        P.tt(hd(Y[:], Y.h[:, :]), yi, W(ec[:], ec.h[:, 0, :].unsqueeze(2).to_broadcast([128, 16, 64])), ALU.mult)
        P.tt(Y[:], ya_, Y[:], ALU.add)
        for g in range(4):
            P.mm(pY[0:64, g // 2, (g % 2) * 256:(g % 2 + 1) * 256], B_c[:, g * 64:(g + 1) * 64], xdd[:, g * 256:(g + 1) * 256])
        P.tt(state[:], state[:], W(ec[:], ec.h[0:64, 1, :].unsqueeze(2).to_broadcast([64, 16, 64])), ALU.mult)
        P.tt(state[:], state[:], W(pY[:], pY.h[0:64, 0:2, :].rearrange("p q (h d) -> p (q h) d", d=64)), ALU.add)
        P.copy(state_bf[:], state[:], eng="act")
        P.tt(hd(Tt[:], Tt.h[:, :]), hd(x_c[:], x_c.h[:, :]),
             W(Dt[:], Dt.h[:, :].unsqueeze(2).to_broadcast([128, 16, 64])), ALU.mult, eng="pool")
        P.tt(Y[:], Y[:], Tt[:], ALU.add, eng="pool")
        P.tt(Y[:], Y[:], z_c[:], ALU.mult, eng="pool")
        P.act(junk[:], Y[:], AF.Square, accum_out=ss[:, 0:1])
        P.act(ss[:, 1:2], ss[:, 0:1], AF.Sqrt, bias=EPS, scale=1.0 / 1024)
        P.recip(ss[:, 1:2], ss[:, 1:2])
        P.stt(Yn[:], Y[:], ss[:, 1:2], ng[:], ALU.mult, ALU.mult)

    def s6(c):
        b = c // 4
        Yn = Yn_[c % 2]
        yb_ = ybT[0]
        for half in range(2):
            for k in range(4):
                kk = half * 4 + k
                P.transpose(W(ptr, ptr.ap[:, k, :]), Yn[:, kk * 128:(kk + 1) * 128], C.identb[:])
            P.copy(yb_[:, half * 4:(half + 1) * 4, (c % 4) * 128:(c % 4 + 1) * 128], ptr, eng="act")
        if c % 4 == 3:
            P.dma(V(mixT.h[512:1536, b * 512:(b + 1) * 512].rearrange("(k p) t -> p k t", p=128), [(mixT.name, b)]), yb_[:])

    pipeline(NT, [s0, s1, s2, s3, s4, s5, s6])
    P.release(m)


LN_G = [math.log(1.0 - 2.0 ** (-5.0 - h)) for h in range(4)]
DK = 128
QSC = DK ** -0.5


def layer1_mixer(C):
    P, I = C.P, C.I
    m0 = P.mark()
    Win = dram2d(I["od_in_w"], 0)
    d_QsT = P.dram("d_QsT", [1024, L], BF16, nsub=4)
    d_KsT = P.dram("d_KsT", [1024, L], BF16, nsub=4)
    d_Kst = P.dram("d_Kst", [L, 1024], BF16, nsub=4)
    d_V = P.dram("d_V", [L, 2048], BF16, nsub=NT)
    d_G = P.dram("d_G", [L, 2048], BF16, nsub=NT)
    mixT = P.dram("d_mixT1", [2048, L], BF16, nsub=4)
    C.mixT = mixT
    eblast = P.sbuf("eblast", [128, NT, 4], F32)
    tri = P.sbuf("tri1", [128, 128], F32)
    P.dma(tri[:], I["c_tri"][:])

    mB = P.mark()
    hnT = P.sbuf("hnT1", [128, KT, L], BF16, nsub=4)
    m2 = P.mark()
    norm_T(C, 1, hnT)
    P.release(m2)

    cosF = P.sbuf("cosF", [128, L], F32)
    sinS = P.sbuf("sinS", [128, L], F32)
    Eq = P.sbuf("Eq", [128, 4, 128], F32)
    Ek = P.sbuf("Ek", [128, 4, 128], F32)
    Es = P.sbuf("Es", [128, 4, 128], F32)
    m2 = P.mark()
    pos = P.sbuf("posf", [128, L], F32)
    P.dma(pos[:], I["c_pos"][:])
    dmod = P.sbuf("dmod", [128, 1], F32)
    sgn = P.sbuf("sgn", [128, 1], F32)
    invf = P.sbuf("invf", [128, 1], F32)
    P.dma(dmod[:], I["c_dmod"][:])
    P.dma(sgn[:], I["c_sign"][:])
    P.act(invf[:], dmod[:], AF.Exp, scale=-math.log(10000.0) / 64.0)
    ang = P.sbuf("angf", [128, L], F32)
    tfb = P.sbuf("tfbf", [128, L], F32)
    tib = P.sbuf("tibf", [128, L], I32)
    rrb = P.sbuf("rrbf", [128, L], F32)
    P.ts(ang[:], pos[:], invf[:, 0:1], ALU.mult, None)
    range_reduce(C, ang[:], tfb[:], tib[:], rrb[:])
    P.act(sinS[:], rrb[:], AF.Sin)
    P.ts(sinS[:], sinS[:], sgn[:, 0:1], ALU.mult, None)
    P.ts(ang[:], ang[:], math.pi / 2, ALU.add, None)
    range_reduce(C, ang[:], tfb[:], tib[:], rrb[:])
    P.act(cosF[:], rrb[:], AF.Sin)
    for h in range(4):
        lg = LN_G[h]
        P.act(Eq[:, h, :], pos[:, 0:128], AF.Exp, scale=lg, bias=lg)
        P.act(Ek[:, h, :], pos[:, 0:128], AF.Exp, scale=-lg, bias=-lg + math.log(QSC))
        P.act(Es[:, h, :], pos[:, 0:128], AF.Exp, scale=-lg, bias=127.0 * lg + math.log(QSC))
    P.release(m2)

    m2 = P.mark()
    wn = [P.sbuf("wn", [128, KT, 128], BF16) for _ in range(2)]
    ws = [P.sbuf("wsw", [128, KT, 128], BF16) for _ in range(2)]
    pn = [P.psum("pn", [128, 512], F32) for _ in range(2)]
    psw = [P.psum("psw", [128, 512], F32) for _ in range(2)]
    ptk = [P.psum("ptk", [128, 4, 128], BF16) for _ in range(2)]
    r1 = [P.sbuf("r1", [128, 512], F32) for _ in range(2)]
    r2 = [P.sbuf("r2", [128, 512], F32) for _ in range(2)]
    ob = [P.sbuf("ob", [128, 512], BF16) for _ in range(4)]
    kst = [P.sbuf("kst", [128, 512], BF16) for _ in range(2)]
    kstg = [P.sbuf("kstg", [128, 4, 128], BF16) for _ in range(2)]
    def e4(tb_, h):
        return W(tb_[:], tb_.h[:, h, :].unsqueeze(1).to_broadcast([128, 4, 128]))

    def o4(o_):
        return W(o_[:], o_.h[:, :].rearrange("p (c t) -> p c t", c=4))

    def f0(it):
        wh, b = it // 4, it % 4
        which, h = wh // 4, wh % 4
        w_ = wn[wh % 2]
        s_ = ws[wh % 2]
        if b == 0:
            if wh == 0:
                load_w(C, w_[:], Win, 0, 128)
            if wh + 1 < 8:
                wh1 = wh + 1
                load_w(C, wn[wh1 % 2][:], Win, (wh1 // 4) * 512 + (wh1 % 4) * 128, 128)
            P.copy(s_[:, :, 0:64], w_[:, :, 64:128], eng="pool")
            P.copy(s_[:, :, 64:128], w_[:, :, 0:64], eng="pool")
        i2 = it % 2
        blk = slice(b * 512, (b + 1) * 512)
        for k in range(KT):
            P.mm(pn[i2][:], w_[:, k, :], hnT.s(b)[:, k, blk], start=(k == 0), stop=(k == KT - 1))
        for k in range(KT):
            P.mm(psw[i2][:], s_[:, k, :], hnT.s(b)[:, k, blk], start=(k == 0), stop=(k == KT - 1))

    def f1(it):
        b = it % 4
        i2 = it % 2
        blk = slice(b * 512, (b + 1) * 512)
        P.tt(r1[i2][:], pn[i2][:], cosF[:, blk], ALU.mult)
        P.tt(r2[i2][:], psw[i2][:], sinS[:, blk], ALU.mult)
        P.tt(r1[i2][:], r1[i2][:], r2[i2][:], ALU.add, eng="pool")

    def f2(it):
        wh, b = it // 4, it % 4
        which, h = wh // 4, wh % 4
        i2 = it % 2
        blk = slice(b * 512, (b + 1) * 512)
        r4 = W(r1[i2][:], r1[i2].h[:, :].rearrange("p (c t) -> p c t", c=4))
        o_ = ob[it % 4]
        if which == 0:
            P.tt(o4(o_), r4, e4(Eq, h), ALU.mult)
            P.dma(d_QsT.s(b)[h * 128:(h + 1) * 128, blk], o_[:])
        else:
            P.tt(o4(o_), r4, e4(Ek, h), ALU.mult)
            P.dma(d_KsT.s(b)[h * 128:(h + 1) * 128, blk], o_[:])
            P.tt(o4(kst[i2]), r4, e4(Es, h), ALU.mult, eng="pool")

    def f3(it):
        wh, b = it // 4, it % 4
        which, h = wh // 4, wh % 4
        if which == 0:
            return
        i2 = it % 2
        blk = slice(b * 512, (b + 1) * 512)
        k_ = kst[i2]
        for j in range(4):
            P.transpose(ptk[i2][:, j, :], k_[:, j * 128:(j + 1) * 128], C.identb[:])
        P.copy(kstg[i2][:], ptk[i2][:], eng="act")
        P.dma(V(d_Kst.h[blk, h * 128:(h + 1) * 128].rearrange("(j p) c -> p j c", p=128), [(d_Kst.name, b)]),
              kstg[i2][:])

    pipeline(32, [f0, f1, f2, f3])
    P.release(m2)

    m2 = P.mark()
    walr = P.sbuf("walr", [128, KT, 16], BF16)
    load_w(C, walr[:], Win, 5120, 16)
    alrT = P.sbuf("alrT", [16, L], F32)
    gwt = P.sbuf("gwt", [16, 512], F32)
    P.dma(gwt[:], I["gla_gate_w"][0])
    nb = P.sbuf("nb", [128, 4], F32)
    P.dma(nb[:], V(I["gla_gate_b"].h[0].rearrange("(h d) -> d h", d=128), [("gla_gate_b",)]), allow_slow_non_contiguous=True)
    P.ts(nb[:], nb[:], -1.0, ALU.mult, None)
    ones = P.sbuf("ones1", [128, 128], F32)
    P.memset(ones[:], 1.0)
    pa = [P.psum("pa", [16, 512], F32) for _ in range(2)]
    for b in range(4):
        blk = slice(b * 512, (b + 1) * 512)
        for k in range(KT):
            P.mm(pa[b % 2][:], walr[:, k, :], hnT.s(b)[:, k, blk], start=(k == 0), stop=(k == KT - 1))
        P.copy(alrT[:, blk], pa[b % 2][:])
    wq = [P.sbuf("wq", [128, KT, 128], BF16) for _ in range(2)]
    wk = [P.sbuf("wk", [128, KT, 128], BF16) for _ in range(2)]
    pl = P.psum("pl", [128, 512], F32)
    pq = P.psum("pq", [128, 512], F32)
    pk = P.psum("pk", [128, 512], F32)
    ptk = [P.psum("ptk2", [128, 4, 128], BF16) for _ in range(2)]
    la = [P.sbuf("la", [128, 512], F32) for _ in range(2)]
    bc = [P.sbuf("bc", [128, 512], F32) for _ in range(2)]
    eb = [P.sbuf("eb", [128, 512], F32) for _ in range(2)]
    enb = [P.sbuf("enb", [128, 512], F32) for _ in range(2)]
    est = [P.sbuf("est", [128, 512], F32) for _ in range(2)]
    ob = [P.sbuf("ob2", [128, 512], BF16) for _ in range(4)]
    kst = [P.sbuf("kst2", [128, 512], BF16) for _ in range(2)]
    kstg = [P.sbuf("kstg2", [128, 4, 128], BF16) for _ in range(2)]
    n = 0
    no = 0
    for h in range(4):
        q_ = wq[h % 2]
        k_w = wk[h % 2]
        load_w(C, q_[:], Win, 3072 + h * 128, 128)
        load_w(C, k_w[:], Win, 3584 + h * 128, 128)
        for b in range(4):
            i2 = n % 2
            n += 1
            blk = slice(b * 512, (b + 1) * 512)
            P.mm(pl[:], gwt[:, h * 128:(h + 1) * 128], alrT[:, blk])
            P.act(la[i2][:], pl[:], AF.Exp, scale=-1.0, bias=nb[:, h:h + 1])
            P.act(la[i2][:], la[i2][:], AF.Ln, bias=1.0)
            P.ts(la[i2][:], la[i2][:], -1.0 / 16.0, ALU.mult, None)
            for c in range(4):
                sl = slice(c * 128, (c + 1) * 128)
                P.scan(bc[i2][:, sl], ones[:], la[i2][:, sl], 0.0)
            P.act(eb[i2][:], bc[i2][:], AF.Exp)
            P.act(enb[i2][:], bc[i2][:], AF.Exp, scale=-1.0)
            for c in range(4):
                sl = slice(c * 128, (c + 1) * 128)
                P.act(est[i2][:, sl], bc[i2][:, sl], AF.Exp, scale=-1.0, bias=bc[i2][:, c * 128 + 127:c * 128 + 128])
            P.act(eblast[:, b * 4:(b + 1) * 4, h], W(bc[i2][:], bc[i2].h[:, :].rearrange("p (c t) -> p c t", c=4)[:, :, 127]), AF.Exp)
            for k in range(KT):
                P.mm(pq[:], q_[:, k, :], hnT.s(b)[:, k, blk], start=(k == 0), stop=(k == KT - 1))
            for k in range(KT):
                P.mm(pk[:], k_w[:, k, :], hnT.s(b)[:, k, blk], start=(k == 0), stop=(k == KT - 1))
            o_ = ob[no % 4]
            no += 1
            P.stt(o_[:], pq[:], QSC, eb[i2][:], ALU.mult, ALU.mult)
            P.dma(d_QsT.s(b)[(4 + h) * 128:(5 + h) * 128, blk], o_[:])
            o_ = ob[no % 4]
            no += 1
            P.tt(o_[:], pk[:], enb[i2][:], ALU.mult)
            P.dma(d_KsT.s(b)[(4 + h) * 128:(5 + h) * 128, blk], o_[:])
            k_ = kst[i2]
            P.tt(k_[:], pk[:], est[i2][:], ALU.mult)
            for j in range(4):
                P.transpose(ptk[i2][:, j, :], k_[:, j * 128:(j + 1) * 128], C.identb[:])
            P.copy(kstg[i2][:], ptk[i2][:], eng="act")
            P.dma(V(d_Kst.h[blk, (4 + h) * 128:(5 + h) * 128].rearrange("(j p) c -> p j c", p=128), [(d_Kst.name, b)]),
                  kstg[i2][:])
    P.release(m2)

    m2 = P.mark()
    ngt = P.sbuf("ngt", [128, 2048], F32)
    P.dma(ngt[:, 0:1024], V(I["ret_norm_g"].h[0].partition_broadcast(128), [("ret_norm_g",)]))
    P.dma(ngt[:, 1024:2048], V(I["gla_norm_g"].h[0].partition_broadcast(128), [("gla_norm_g",)]))
    wv = [P.sbuf("wv", [128, KT, 512], BF16) for _ in range(2)]
    pv = [P.psum("pv", [128, 512], F32) for _ in range(4)]
    vs = [P.sbuf("vs", [128, 512], BF16) for _ in range(3)]
    sgt = [P.sbuf("sgt", [128, 512], F32) for _ in range(2)]
    jobs = [(1024, 0, 0), (1536, 0, 512), (4096, 0, 1024), (4608, 0, 1536),
            (2048, 1, 0), (2560, 1, 512), (5136, 1, 1024), (5648, 1, 1536)]
    n = 0
    load_w(C, wv[0][:], Win, jobs[0][0], 512)
    for ji, (c0, isg, dc) in enumerate(jobs):
        w_ = wv[ji % 2]
        if ji + 1 < len(jobs):
            load_w(C, wv[(ji + 1) % 2][:], Win, jobs[ji + 1][0], 512)
        for t in range(NT):
            pp = pv[n % 4]
            o_ = vs[n % 3]
            s_ = sgt[n % 2]
            n += 1
            tok = slice(t * 128, (t + 1) * 128)
            for k in range(KT):
                P.mm(pp[:], hnT.s(t // 4)[:, k, tok], w_[:, k, :], start=(k == 0), stop=(k == KT - 1))
            if isg == 0:
                if n % 2 == 0:
                    P.copy(o_[:], pp[:], eng="act")
                else:
                    P.copy(o_[:], pp[:])
                P.dma(d_V.s(t)[tok, dc:dc + 512], o_[:])
            else:
                P.act(s_[:], pp[:], AF.Silu)
                P.tt(o_[:], s_[:], ngt[:, dc:dc + 512], ALU.mult)
                P.dma(d_G.s(t)[tok, dc:dc + 512], o_[:])
    P.release(m2)
    P.release(mB)

    wo1 = out_proj_load(C, dram2d(I["od_out_w"], 0), 16)
    mC = P.mark()
    state = [P.sbuf("st1", [128, 4, 256], F32) for _ in range(2)]
    state_bf = [P.sbuf("st1b", [128, 4, 256], BF16) for _ in range(2)]
    for mx in range(2):
        P.memset(state[mx][:], 0.0)
        P.memset(state_bf[mx][:], 0.0)
    NS = 3
    Qc = [P.sbuf("Qc", [128, 8, 128], BF16) for _ in range(NS)]
    Kc = [P.sbuf("Kc", [128, 8, 128], BF16) for _ in range(NS)]
    Ksc = [P.sbuf("Ksc", [128, 1024], BF16) for _ in range(NS)]
    Vc = [P.sbuf("Vc", [128, 2048], BF16) for _ in range(NS)]
    Gc = [P.sbuf("Gc", [128, 2048], BF16) for _ in range(NS)]
    att = [P.sbuf("att", [128, 4, 128], BF16) for _ in range(3)]
    oS = [P.sbuf("oS", [128, 4, 256], F32) for _ in range(3)]
    sq = [P.sbuf("sq1", [128, 4, 256], F32) for _ in range(2)]
    on = [P.sbuf("on1", [128, 4, 256], F32) for _ in range(2)]
    fin = [P.sbuf("fin1", [128, 1024], BF16) for _ in range(3)]
    st = [P.sbuf("stat", [128, 4, 4], F32) for _ in range(4)]
    stg = [P.sbuf("stg1", [128, 16, 512], BF16) for _ in range(1)]
    pat = [P.psum("pat", [128, 4, 128], F32) for _ in range(2)]
    pout = P.psum("pout", [128, 2, 512], F32)
    pstt = P.psum("pstt", [128, 2, 512], F32)
    ptr = P.psum("ptr1", [128, 8, 128], BF16)
    tri_b = W(tri[:], tri.h[:, :].unsqueeze(1).to_broadcast([128, 4, 128]))

    def ph0(it):
        c, mx = it // 2, it % 2
        if mx:
            return
        s_ = c % NS
        b = c // 4
        tok = slice(c * 128, (c + 1) * 128)
        P.dma(Qc[s_][:], V(d_QsT.h[:, tok].rearrange("(k p) t -> p k t", p=128), [(d_QsT.name, b)]))
        P.dma(Kc[s_][:], V(d_KsT.h[:, tok].rearrange("(k p) t -> p k t", p=128), [(d_KsT.name, b)]))
        P.dma(Ksc[s_][:], d_Kst.s(b)[tok, :])
        P.dma(Vc[s_][:], d_V.s(c)[tok, :])
        P.dma(Gc[s_][:], d_G.s(c)[tok, :])

    def ph1(it):
        c, mx = it // 2, it % 2
        s_ = c % NS
        for h in range(4):
            P.mm(pat[it % 2][:, h, :], Kc[s_][:, mx * 4 + h, :], Qc[s_][:, mx * 4 + h, :])
        P.tt(att[it % 3][:], pat[it % 2][:], tri_b, ALU.mult)

    def ph2(it):
        c, mx = it // 2, it % 2
        s_ = c % NS
        for h in range(4):
            ov = pout[:, h // 2, (h % 2) * 256:(h % 2 + 1) * 256]
            P.mm(ov, att[it % 3][:, h, :], Vc[s_][:, mx * 1024 + h * 256:mx * 1024 + (h + 1) * 256], start=True, stop=False)
            P.mm(ov, Qc[s_][:, mx * 4 + h, :], state_bf[mx][:, h, :], start=False, stop=True)
        for h in range(4):
            P.mm(pstt[:, h // 2, (h % 2) * 256:(h % 2 + 1) * 256], Ksc[s_][:, mx * 512 + h * 128:mx * 512 + (h + 1) * 128],
                 Vc[s_][:, mx * 1024 + h * 256:mx * 1024 + (h + 1) * 256])

    def ph3(it):
        c, mx = it // 2, it % 2
        o_ = oS[it % 3]
        P.copy(o_[:], W(pout[:], pout.h[:, :, :].rearrange("p q (h d) -> p (q h) d", d=256)), eng="act")
        for h in range(4):
            sp = pstt[:, h // 2, (h % 2) * 256:(h % 2 + 1) * 256]
            if mx == 0:
                P.stt(state[mx][:, h, :], state[mx][:, h, :], math.exp(LN_G[h] * 128.0), sp, ALU.mult, ALU.add)
            else:
                P.stt(state[mx][:, h, :], state[mx][:, h, :], eblast[:, c, h:h + 1], sp, ALU.mult, ALU.add)
        P.copy(state_bf[mx][:], state[mx][:], eng="act")

    def ph4(it):
        c, mx = it // 2, it % 2
        o_ = oS[it % 3]
        st_ = st[it % 4]
        sq_ = sq[it % 2]
        P.act(sq_[:], o_[:], AF.Square)
        P.add("dve", (lambda sq_=sq_, st_=st_: C.nc.vector.tensor_reduce(st_.h[:, :, 1], sq_.h[:, :, :], AX.X, ALU.add)), [sq_[:]], [st_[:]])
        if mx == 0:
            P.add("dve", (lambda o_=o_, st_=st_: C.nc.vector.tensor_reduce(st_.h[:, :, 0], o_.h[:, :, :], AX.X, ALU.add)), [o_[:]], [st_[:]])
            P.ts(st_[:, :, 0], st_[:, :, 0], 1.0 / 256, ALU.mult, None)
            P.tt(st_[:, :, 2], st_[:, :, 0], st_[:, :, 0], ALU.mult)
            P.stt(st_[:, :, 1], st_[:, :, 1], 1.0 / 256, st_[:, :, 2], ALU.mult, ALU.subtract)
        else:
            P.ts(st_[:, :, 1], st_[:, :, 1], 1.0 / 256, ALU.mult, None)
        P.act(st_[:, :, 3], st_[:, :, 1], AF.Sqrt, bias=EPS)
        P.recip(st_[:, :, 3], st_[:, :, 3])

    def ph5(it):
        c, mx = it // 2, it % 2
        s_ = c % NS
        o_ = oS[it % 3]
        st_ = st[it % 4]
        on_ = on[it % 2]
        fin_ = fin[it % 3]
        rb = W(st_[:], st_.h[:, :, 3:4].to_broadcast([128, 4, 256]))
        if mx == 0:
            mb_ = W(st_[:], st_.h[:, :, 0:1].to_broadcast([128, 4, 256]))
            P.tt(on_[:], o_[:], mb_, ALU.subtract, eng="pool")
            P.tt(on_[:], on_[:], rb, ALU.mult, eng="pool")
        else:
            P.tt(on_[:], o_[:], rb, ALU.mult, eng="pool")
        P.tt(fin_[:], W(on_[:], on_.h[:, :, :].rearrange("p h d -> p (h d)")), Gc[s_][:, mx * 1024:(mx + 1) * 1024], ALU.mult)

    def ph6(it):
        c, mx = it // 2, it % 2
        b = c // 4
        fin_ = fin[it % 3]
        stg_ = stg[0]
        for k in range(8):
            P.transpose(ptr[:, k, :], fin_[:, k * 128:(k + 1) * 128], C.identb[:])
        P.copy(stg_[:, mx * 8:(mx + 1) * 8, (c % 4) * 128:(c % 4 + 1) * 128], ptr[:], eng="act")
        if c % 4 == 3 and mx == 1:
            P.dma(V(mixT.h[:, b * 512:(b + 1) * 512].rearrange("(k p) t -> p k t", p=128), [(mixT.name, b)]), stg_[:])

    pipeline(2 * NT, [ph0, ph1, ph2, ph3, ph4, ph5, ph6])
    P.release(mC)
    if "yc" in C.tap_req:
        tap(C, "yc", mixT[:], [2048, L], BF16)
        P.flush(barrier=True)
    if C.stop_after == "l1mix":
        P.release(m0)
        return
    out_proj_run(C, wo1, 16)
    P.release(m0)
```
